# Optimizing a Trainium2 kernel written in Bass

```python
import jax, jax.numpy as jnp
from jax import lax
import numpy as np

D_MODEL = 1024
BATCH = 32
SEQ = 2048
DEPTH = 1

D_MIX = D_MODEL
POOL_WIDTH = D_MIX // 4
POOL_WINDOWS = (2, 4, 8, 16)
N_POOL_GROUPS = len(POOL_WINDOWS)
POOL_GROUP_DIM = POOL_WIDTH // N_POOL_GROUPS
SSD_WIDTH = D_MIX - POOL_WIDTH
SSD_HEAD_DIM = 64
SSD_HEADS = SSD_WIDTH // SSD_HEAD_DIM
SSD_GROUPS = 4
SSD_HEADS_PER_GROUP = SSD_HEADS // SSD_GROUPS
SSD_STATE = 128
SSD_CONV = 4
SSD_CHUNK = 128
SSD_CONV_DIM = SSD_WIDTH + 2 * SSD_GROUPS * SSD_STATE
IN_PROJ_DIM = POOL_WIDTH + SSD_WIDTH + SSD_CONV_DIM + SSD_HEADS
N_EXPERT_GROUPS = 4
EXPERTS_PER_GROUP = 8
N_EXPERTS = N_EXPERT_GROUPS * EXPERTS_PER_GROUP
TOP_K = 2
EXPERT_FF = 512
MOE_BLOCK = 128
NORM_EPS = 1e-6

kernel_name = "hymba_pool_ssd_hmoe_adaln"


def rms_norm(x, eps=NORM_EPS):
    xf = x.astype(jnp.float32)
    y = xf * lax.rsqrt(jnp.mean(xf * xf, axis=-1, keepdims=True) + eps)
    return y.astype(x.dtype)


def modulate(x, shift, scale):
    return rms_norm(x) * (1 + scale[:, None, :]) + shift[:, None, :]


def pool_mixer(u, w_pool, pool_scale):
    b, s, _ = u.shape
    ug = u.reshape(b, s, N_POOL_GROUPS, POOL_GROUP_DIM)
    cs = jnp.cumsum(ug.astype(jnp.float32), axis=1)
    pos = jnp.arange(1, s + 1, dtype=jnp.float32)
    means = []
    for g, w in enumerate(POOL_WINDOWS):
        csg = cs[:, :, g]
        lagged = jnp.pad(csg, ((0, 0), (w, 0), (0, 0)))[:, :s]
        cnt = jnp.minimum(pos, float(w))
        means.append((csg - lagged) / cnt[None, :, None])
    mean = jnp.stack(means, axis=2).astype(u.dtype)
    y = jnp.einsum('bsgc,gcd->bsgd', mean - ug, w_pool)
    return y.reshape(b, s, POOL_WIDTH) * pool_scale


def causal_depthwise_conv(x, w, bias):
    ch = x.shape[-1]
    y = lax.conv_general_dilated(
        x, w[:, None, :].astype(x.dtype), window_strides=(1,),
        padding=((SSD_CONV - 1, 0),), dimension_numbers=('NWC', 'WIO', 'NWC'),
        feature_group_count=ch)
    return y + bias


def ssd_scan(xh, dt, a, bmat, cmat):
    b, s = xh.shape[:2]
    L = SSD_CHUNK
    nc = s // L
    G, R, P, N = SSD_GROUPS, SSD_HEADS_PER_GROUP, SSD_HEAD_DIM, SSD_STATE
    xc = (xh.astype(jnp.float32) * dt[..., None]).reshape(b, nc, L, G, R, P)
    bc = bmat.astype(jnp.float32).reshape(b, nc, L, G, N)
    cc = cmat.astype(jnp.float32).reshape(b, nc, L, G, N)
    a_cs = jnp.cumsum((dt * a).reshape(b, nc, L, G, R), axis=2)
    causal = jnp.tril(jnp.ones((L, L), dtype=bool))
    seg = a_cs[:, :, :, None] - a_cs[:, :, None, :]
    decay = jnp.exp(jnp.where(causal[None, None, :, :, None, None], seg, -jnp.inf))
    cb = jnp.einsum('bclgn,bcsgn->bclsg', cc, bc)
    y_diag = jnp.einsum('bclsgr,bcsgrp->bclgrp', cb[..., None] * decay, xc)
    decay_to_end = jnp.exp(a_cs[:, :, -1:] - a_cs)
    chunk_states = jnp.einsum('bclgn,bclgrp->bcgrpn', bc, xc * decay_to_end[..., None])
    chunk_decay = jnp.exp(a_cs[:, :, -1])

    def step(state, inp):
        st, dec = inp
        return state * dec[..., None, None] + st, state

    init = jnp.zeros((b, G, R, P, N), jnp.float32)
    _, prev = lax.scan(step, init, (jnp.moveaxis(chunk_states, 1, 0),
                                    jnp.moveaxis(chunk_decay, 1, 0)))
    prev = jnp.moveaxis(prev, 0, 1)
    y_off = jnp.einsum('bclgn,bcgrpn->bclgrp', cc, prev) * jnp.exp(a_cs)[..., None]
    return (y_diag + y_off).reshape(b, s, G, R, P)


def ssd_mixer(z, xbc, dt_raw, conv_w, conv_b, dt_bias, a_log, d_skip, norm_w):
    b, s, _ = z.shape
    G, R, P, N = SSD_GROUPS, SSD_HEADS_PER_GROUP, SSD_HEAD_DIM, SSD_STATE
    xbc = jax.nn.silu(causal_depthwise_conv(xbc, conv_w, conv_b))
    xs, bm, cm = jnp.split(xbc, [SSD_WIDTH, SSD_WIDTH + G * N], axis=-1)
    xh = xs.reshape(b, s, G, R, P)
    bm = bm.reshape(b, s, G, N)
    cm = cm.reshape(b, s, G, N)
    dt = jax.nn.softplus(dt_raw.astype(jnp.float32) + dt_bias.astype(jnp.float32)).reshape(b, s, G, R)
    a = -jnp.exp(a_log.astype(jnp.float32)).reshape(G, R)
    y = ssd_scan(xh, dt, a, bm, cm) + xh.astype(jnp.float32) * d_skip.astype(jnp.float32).reshape(G, R)[..., None]
    y = y.reshape(b, s, SSD_WIDTH).astype(z.dtype) * jax.nn.silu(z)
    y = rms_norm(y.reshape(b, s, G, SSD_WIDTH // G)).reshape(b, s, SSD_WIDTH)
    return y * norm_w


def hier_moe(h, w_group, b_group, w_router, b_router, w13, w2):
    b, s, d = h.shape
    T = b * s
    hf = h.reshape(T, d)
    g_logits = (hf @ w_group + b_group).astype(jnp.float32)
    g_prob = jax.nn.softmax(g_logits, axis=-1)
    p_g, g_idx = lax.top_k(g_prob, 1)
    e_logits = (hf @ w_router + b_router).astype(jnp.float32).reshape(T, N_EXPERT_GROUPS, EXPERTS_PER_GROUP)
    within = jnp.take_along_axis(e_logits, g_idx[:, :, None], axis=1)[:, 0]
    top_v, top_i = lax.top_k(within, TOP_K)
    gate = p_g * jax.nn.softmax(top_v, axis=-1)
    expert_id = (g_idx * EXPERTS_PER_GROUP + top_i).astype(jnp.int32)
    A = T * TOP_K
    flat_e = expert_id.reshape(A)
    flat_tok = jnp.repeat(jnp.arange(T, dtype=jnp.int32), TOP_K)
    flat_w = gate.reshape(A)
    order = jnp.argsort(flat_e)
    se, stok, sw = flat_e[order], flat_tok[order], flat_w[order]
    counts = jnp.bincount(flat_e, length=N_EXPERTS)
    starts = jnp.cumsum(counts) - counts
    padded = ((counts + MOE_BLOCK - 1) // MOE_BLOCK) * MOE_BLOCK
    pends = jnp.cumsum(padded)
    pstarts = pends - padded
    dest = pstarts[se] + (jnp.arange(A, dtype=jnp.int32) - starts[se])
    R = A + N_EXPERTS * MOE_BLOCK
    n_blk = R // MOE_BLOCK
    row_tok = jnp.zeros((R,), jnp.int32).at[dest].set(stok)
    row_w = jnp.zeros((R,), h.dtype).at[dest].set(sw.astype(h.dtype))
    blk_start = jnp.arange(n_blk, dtype=jnp.int32) * MOE_BLOCK
    blk_e = jnp.clip(jnp.searchsorted(pends, blk_start, side='right'), 0, N_EXPERTS - 1)
    xs = hf[row_tok].reshape(n_blk, MOE_BLOCK, d)

    def expert_block(args):
        xb, e = args
        hu = xb @ w13[e]
        a_, b_ = jnp.split(hu, 2, axis=-1)
        return (jax.nn.silu(a_) * b_) @ w2[e]

    ys = lax.map(expert_block, (xs, blk_e)).reshape(R, d)
    out = jax.ops.segment_sum(ys * row_w[:, None], row_tok, num_segments=T)
    return out.reshape(b, s, d)


def setup_inputs(seed: int = 0) -> dict:
    key = jax.random.key(seed)
    ks = jax.random.split(key, 24)
    f32 = jnp.float32
    nrm = lambda k, shape, scale: jax.random.normal(k, shape, f32) * scale
    x = jax.random.normal(ks[0], (BATCH, SEQ, D_MODEL), f32)
    c = jax.random.normal(ks[1], (BATCH, D_MODEL), f32)
    w_ada = nrm(ks[2], (DEPTH, D_MODEL, 6 * D_MODEL), D_MODEL ** -0.5)
    b_ada = nrm(ks[3], (DEPTH, 6 * D_MODEL), 0.02)
    w_in = nrm(ks[4], (DEPTH, D_MODEL, IN_PROJ_DIM), D_MODEL ** -0.5)
    w_pool = nrm(ks[5], (DEPTH, N_POOL_GROUPS, POOL_GROUP_DIM, POOL_GROUP_DIM), POOL_GROUP_DIM ** -0.5)
    pool_scale = 1.0 + nrm(ks[6], (DEPTH, POOL_WIDTH), 0.1)
    conv_w = nrm(ks[7], (DEPTH, SSD_CONV, SSD_CONV_DIM), SSD_CONV ** -0.5)
    conv_b = nrm(ks[8], (DEPTH, SSD_CONV_DIM), 0.02)
    dt0 = jnp.exp(jax.random.uniform(ks[9], (DEPTH, SSD_HEADS), f32, np.log(1e-3), np.log(1e-1)))
    dt_bias = dt0 + jnp.log(-jnp.expm1(-dt0))
    a_log = jnp.log(jax.random.uniform(ks[10], (DEPTH, SSD_HEADS), f32, 1.0, 16.0))
    d_skip = 1.0 + nrm(ks[11], (DEPTH, SSD_HEADS), 0.1)
    ssd_norm_w = 1.0 + nrm(ks[12], (DEPTH, SSD_WIDTH), 0.1)
    w_out = nrm(ks[13], (DEPTH, D_MIX, D_MODEL), D_MIX ** -0.5)
    w_group = nrm(ks[14], (DEPTH, D_MODEL, N_EXPERT_GROUPS), D_MODEL ** -0.5)
    b_group = nrm(ks[15], (DEPTH, N_EXPERT_GROUPS), 0.01)
    w_router = nrm(ks[16], (DEPTH, D_MODEL, N_EXPERTS), D_MODEL ** -0.5)
    b_router = nrm(ks[17], (DEPTH, N_EXPERTS), 0.01)
    w13 = nrm(ks[18], (DEPTH, N_EXPERTS, D_MODEL, 2 * EXPERT_FF), D_MODEL ** -0.5)
    w2 = nrm(ks[19], (DEPTH, N_EXPERTS, EXPERT_FF, D_MODEL), EXPERT_FF ** -0.5)
    final_norm_w = 1.0 + nrm(ks[20], (D_MODEL,), 0.1)
    return {"x": x, "c": c, "w_ada": w_ada, "b_ada": b_ada, "w_in": w_in,
            "w_pool": w_pool, "pool_scale": pool_scale, "conv_w": conv_w, "conv_b": conv_b,
            "dt_bias": dt_bias, "a_log": a_log, "d_skip": d_skip, "ssd_norm_w": ssd_norm_w,
            "w_out": w_out, "w_group": w_group, "b_group": b_group, "w_router": w_router,
            "b_router": b_router, "w13": w13, "w2": w2, "final_norm_w": final_norm_w}


def reference(x, c, w_ada, b_ada, w_in, w_pool, pool_scale, conv_w, conv_b, dt_bias, a_log,
              d_skip, ssd_norm_w, w_out, w_group, b_group, w_router, b_router, w13, w2,
              final_norm_w):
    split_at = [POOL_WIDTH, POOL_WIDTH + SSD_WIDTH, POOL_WIDTH + SSD_WIDTH + SSD_CONV_DIM]
    for layer in range(DEPTH):
        mod = jax.nn.silu(c) @ w_ada[layer] + b_ada[layer]
        sh1, sc1, g1, sh2, sc2, g2 = jnp.split(mod, 6, axis=-1)
        h = modulate(x, sh1, sc1)
        proj = h @ w_in[layer]
        u, z, xbc, dt_raw = jnp.split(proj, split_at, axis=-1)
        y_pool = pool_mixer(u, w_pool[layer], pool_scale[layer])
        y_ssd = ssd_mixer(z, xbc, dt_raw, conv_w[layer], conv_b[layer], dt_bias[layer],
                          a_log[layer], d_skip[layer], ssd_norm_w[layer])
        mix = jnp.concatenate([y_pool, y_ssd], axis=-1) @ w_out[layer]
        x = x + g1[:, None, :] * mix
        h = modulate(x, sh2, sc2)
        x = x + g2[:, None, :] * hier_moe(h, w_group[layer], b_group[layer], w_router[layer],
                                           b_router[layer], w13[layer], w2[layer])
    return rms_norm(x) * final_norm_w
```

```python
import numpy as np
import ml_dtypes
from contextlib import ExitStack
import concourse.bass as bass
import concourse.mybir as mybir
from concourse.bass_utils import run_bass_kernel_spmd

F32 = mybir.dt.float32
BF16 = mybir.dt.bfloat16
I32 = mybir.dt.int32
ALU = mybir.AluOpType
AF = mybir.ActivationFunctionType
AX = mybir.AxisListType

D = 1024
KC = 8
INP = 2828
NE = 32
FF = 512
NCORES = 8
EPS = 1e-6
BIG = 30000.0
C_U, C_XBC, C_Z, C_DT = 0, 256, 2048, 2816


class _Op:
    __slots__ = ("eng", "fn", "deps", "dma", "dma_idx", "sig", "sigval", "idx", "waits", "dma_need")


class _Rec:
    def __getattr__(self, name):
        def f(*a, **k):
            self.__dict__["call"] = (name, a, k)
            return self
        return f


class Sched:
    ENGS = ("pe", "act", "dve", "pool", "sp")

    def __init__(self):
        self.ops = {e: [] for e in self.ENGS}
        self.last_w = {}
        self.readers = {}
        self.dma_count = {}
        self.dma_last = {}
        self.pending = {e: [] for e in self.ENGS}

    def add(self, eng, fn, r=(), w=(), dma=None):
        o = _Op()
        rec = _Rec()
        fn(rec)
        o.eng, o.fn, o.dma, o.sig, o.sigval, o.dma_idx = eng, rec.call, dma, False, 0, 0
        deps = [(d, True) for d in self.pending[eng]]
        self.pending[eng] = []
        for res in r:
            lw = self.last_w.get(res)
            if lw is not None:
                deps.append((lw, True))
        for res in w:
            rd = self.readers.get(res, ())
            lw = self.last_w.get(res)
            if lw is not None and not rd:
                deps.append((lw, False))
            deps.extend((x, False) for x in rd)
        o.deps = [(d, raw) for d, raw in deps if d.dma is None]
        o.dma_need = {d.dma: self.dma_count[d.dma] for d, raw in deps if d.dma is not None}
        if dma is not None:
            self.dma_count[dma] = self.dma_count.get(dma, 0) + 1
            o.dma_idx = self.dma_count[dma]
            self.dma_last[dma] = o
        for res in r:
            self.readers.setdefault(res, []).append(o)
        for res in w:
            self.last_w[res] = o
            self.readers[res] = []
        o.idx = len(self.ops[eng])
        self.ops[eng].append(o)
        return o

    def barrier(self):
        deps = []
        for e in self.ENGS:
            if self.ops[e]:
                deps.append(self.ops[e][-1])
        deps.extend(self.dma_last.values())
        for e in self.ENGS:
            self.pending[e] = list(deps)

    def finalize(self):
        for e in self.ENGS:
            seen_eng = {}
            seen_dma = {}
            for o in self.ops[e]:
                need_eng = {}
                need_dma = dict(o.dma_need)
                for d, raw in o.deps:
                    if d is o:
                        continue
                    if d.dma is not None:
                        if d.dma_idx > need_dma.get(d.dma, 0):
                            need_dma[d.dma] = d.dma_idx
                    else:
                        if d.eng == e and e == "pe":
                            continue
                        if d.eng == e and d.idx >= o.idx:
                            continue
                        cur = need_eng.get(d.eng)
                        if cur is None or d.idx > cur.idx:
                            need_eng[d.eng] = d
                waits = []
                for k, v in need_dma.items():
                    if v > seen_dma.get(k, 0):
                        seen_dma[k] = v
                        waits.append(("dma", k, v))
                for k, d in need_eng.items():
                    if d.idx > seen_eng.get(k, -1):
                        seen_eng[k] = d.idx
                        d.sig = True
                        waits.append(("eng", k, d))
                o.waits = waits
        self.n_epochs = {}
        for e in self.ENGS:
            c = 0
            for o in self.ops[e]:
                if o.sig and o.dma is None:
                    c += 1
                    o.sigval = c
            self.n_epochs[e] = (c + self.EP - 1) // self.EP if c else 0

    EP = 1000
    DEP = 64

    def emit(self, nc, stack):
        self.finalize()
        esem = {e: [stack.enter_context(nc.semaphore("s_%s%d" % (e, i))) for i in range(self.n_epochs[e])]
                for e in self.ENGS}
        dsem = {k: [stack.enter_context(nc.semaphore("d_%s_%d" % (k, i))) for i in range((n + self.DEP - 1) // self.DEP)]
                for k, n in self.dma_count.items()}
        block = stack.enter_context(nc.Block())
        EP, DEP = self.EP, self.DEP

        def run(e, h):
            for o in self.ops[e]:
                for kind, k, v in o.waits:
                    if kind == "dma":
                        h.wait_ge(dsem[k][(v - 1) // DEP], 16 * ((v - 1) % DEP + 1))
                    else:
                        h.wait_ge(esem[k][(v.sigval - 1) // EP], (v.sigval - 1) % EP + 1)
                name, a, k = o.fn
                try:
                    ins = getattr(h, name)(*a, **k)
                except Exception:
                    print("EMIT FAIL", e, name, {kk: str(vv)[:300] for kk, vv in k.items()}, flush=True)
                    raise
                if o.dma is not None:
                    ins.then_inc(dsem[o.dma][(o.dma_idx - 1) // DEP], 16)
                elif o.sig:
                    ins.then_inc(esem[e][(o.sigval - 1) // EP], 1)

        @block.tensor
        def _(h):
            run("pe", h)

        @block.scalar
        def _(h):
            run("act", h)

        @block.vector
        def _(h):
            run("dve", h)

        @block.gpsimd
        def _(h):
            run("pool", h)

        @block.sync
        def _(h):
            run("sp", h)


def build(nseq=4, seqlen=2048, debug=()):
    T = nseq * seqlen
    NT = T // 128
    TM = 256
    CPM = TM // 128
    NMT = seqlen // TM
    R = 2 * T + NE * 128
    NB = R // 128
    nc = bass.Bass("TRN2", target_bir_lowering=False)
    S = Sched()
    st = ExitStack()

    def din(name, shape, dt=F32):
        return nc.dram_tensor(name, list(shape), dt, kind="ExternalInput").ap()

    x_d = din("x", [T, D])
    cT_d = din("cT", [128, KC, nseq])
    wada_d = din("w_ada", [D, 6 * D])
    badaT_d = din("b_adaT", [128, 48])
    win_d = din("w_in", [D, INP])
    wpool_d = din("w_pool", [4, 64, 64])
    pscale_d = din("pscale", [128, 2])
    convw_d = din("convw", [128, 56])
    convb_d = din("convb", [128, 14])
    dtb_d = din("dtb", [128, 12])
    alog_d = din("alog", [128, 12])
    dskip_d = din("dskip", [128, 12])
    normw_d = din("normw", [128, 6])
    wout_d = din("w_out", [D, D])
    wgr_d = din("wgr", [128, KC, 36])
    bgr_d = din("bgr", [128, 36])
    w13_d = din("w13", [NE, D, 2 * FF])
    w2_d = din("w2", [NE, FF, D])
    fw_d = din("fw", [128, D])
    pcoef_d = din("pcoef", [128, 2, 4])
    pratio_d = din("pratio", [128, 2, 16])
    out_d = nc.dram_tensor("out", [T, D], F32, kind="ExternalOutput").ap()
    x1s_d = nc.dram_tensor("x1s", [T, D], F32).ap()
    h2s_d = nc.dram_tensor("h2s", [T, D], BF16).ap()
    xs_d = nc.dram_tensor("xs", [R, D], BF16).ap()
    ys_d = nc.dram_tensor("ys", [R, D], F32).ap()
    dbg_out = {}

    def sb(name, shape, dt=F32, stack=st):
        return stack.enter_context(nc.sbuf_tensor("sb_" + name, list(shape), dt))

    def ps(name, shape, dt=F32):
        return st.enter_context(nc.psum_tensor("ps_" + name, list(shape), dt))

    def dma(eng, out, in_, r, w, key):
        return S.add(eng, lambda h, o=out, i=in_: h.dma_start(out=o, in_=i), r=r, w=w, dma=key)

    def dump(name, ap, shape, res, dt=F32):
        d = nc.dram_tensor("dbg_" + name, list(shape), dt, kind="ExternalOutput").ap()
        dbg_out[name] = d
        dma("sp", d, ap, res, [], "dbg")

    def finish():
        S.barrier()
        S.add("sp", lambda h: h.nop(), r=[], w=[])
        S.emit(nc, st)
        return nc, dbg_out

    psT = ps("psT", [128, 512])
    psW = ps("psW", [128, 1024])
    psSG = ps("psSG", [128, 1024])
    psY = ps("psY", [128, 1024])
    psS = ps("psS", [128, 512])
    psT_bf = psT[:, :].bitcast(BF16)

    ident_f = sb("ident_f", [128, 128])
    ident_b = sb("ident_b", [128, 128], BF16)
    ones_f = sb("ones_f", [128, 128])
    ones_b = sb("ones_b", [128, 128], BF16)
    mJL = sb("mJL", [128, 128])
    mST = sb("mST", [128, 128])
    mLT_b = sb("mLT_b", [128, 128], BF16)
    iota_p = sb("iota_p", [128, 1])
    iota_e = sb("iota_e", [128, 32])
    modp = sb("modp", [128, 48, nseq])
    gates = sb("gates", [128, NT, 2])
    eidx = sb("eidx", [128, NT, 2])
    posk = sb("posk", [128, NT, 2])
    totrun = sb("totrun", [128, 32])
    dest_i = sb("dest_i", [128, NT, 2], I32)
    widx13_i = sb("widx13_i", [128, NB, 4], I32)
    widx2_i = sb("widx2_i", [128, NB, 2], I32)
    fwrep = sb("fwrep", [128, D])

    stA = ExitStack()

    def sba(name, shape, dt=F32):
        return sb(name, shape, dt, stack=stA)

    win_b = sba("win_b", [128, KC, INP], BF16)
    wout_b = sba("wout_b", [128, KC, D], BF16)
    cdiag = sba("cdiag", [128, 56, 128], BF16)
    ddiag = sba("ddiag", [128, 12, 128], BF16)
    wpool_b = sba("wpool_b", [128, 2, 128], BF16)
    wgr = sba("wgr", [128, KC, 36])
    bgr = sba("bgr", [128, 36])
    pscale = sba("pscale", [128, 2])
    convw = sba("convw", [128, 56])
    convb = sba("convb", [128, 14])
    dtb = sba("dtb", [128, 12])
    arep = sba("arep", [128, 12])
    dskip = sba("dskip", [128, 12])
    normw = sba("normw", [128, 6])
    pcoef = sba("pcoef", [128, 2, 4])
    pratio = sba("pratio", [128, 2, 16])
    cT = sba("cT", [128, KC, nseq])
    badaT = sba("badaT", [128, 48])
    stI = ExitStack()
    stage = sb("stage", [128, 6144], stack=stI)
    stage2 = sb("stage2", [128, 6144], stack=stI)

    for t_, d_ in ((wgr, wgr_d), (bgr, bgr_d), (pscale, pscale_d), (convb, convb_d), (dtb, dtb_d), (arep, alog_d),
                   (dskip, dskip_d), (normw, normw_d), (pcoef, pcoef_d), (pratio, pratio_d), (convw, convw_d),
                   (cT, cT_d), (badaT, badaT_d), (fwrep, fw_d)):
        dma("sp", t_[:], d_, [], [], "init")

    S.add("pool", lambda h: h.memset(ones_f[:], 1.0), w=["ones_f"])
    S.add("pool", lambda h: h.memset(ones_b[:], 1.0), w=["ones_b"])
    S.add("pool", lambda h: h.affine_select(out=ident_f[:], in_=ones_f[:], pattern=[[-1, 128]],
                                            compare_op=ALU.is_equal, fill=0.0, base=0, channel_multiplier=1),
          r=["ones_f"], w=["ident_f"])
    S.add("pool", lambda h: h.affine_select(out=mJL[:], in_=ones_f[:], pattern=[[1, 128]],
                                            compare_op=ALU.is_ge, fill=0.0, base=0, channel_multiplier=-1),
          r=["ones_f"], w=["mJL"])
    S.add("pool", lambda h: h.affine_select(out=mST[:], in_=ones_f[:], pattern=[[-1, 128]],
                                            compare_op=ALU.is_gt, fill=0.0, base=0, channel_multiplier=1),
          r=["ones_f"], w=["mST"])
    S.add("pool", lambda h: h.affine_select(out=mLT_b[:], in_=ones_b[:], pattern=[[1, 128]],
                                            compare_op=ALU.is_gt, fill=0.0, base=0, channel_multiplier=-1),
          r=["ones_b"], w=["mLT_b"])
    S.add("pool", lambda h: h.tensor_copy(out=ident_b[:], in_=ident_f[:]), r=["ident_f"], w=["ident_b"])
    S.add("pool", lambda h: h.iota(iota_p[:], pattern=[[0, 1]], base=0, channel_multiplier=1,
                                   allow_small_or_imprecise_dtypes=True), w=["iota_p"])
    S.add("pool", lambda h: h.iota(iota_e[:], pattern=[[1, 32]], base=0, channel_multiplier=0,
                                   allow_small_or_imprecise_dtypes=True), w=["iota_e"])
    S.add("pool", lambda h: h.memset(totrun[:], 0.0), w=["totrun"])
    S.add("pool", lambda h: h.memset(stage2[:, 0:256], 0.0), w=["stage2"])
    S.barrier()

    S.add("act", lambda h: h.activation(out=arep[:], in_=arep[:], func=AF.Exp), r=[], w=["arep"])
    S.add("dve", lambda h: h.tensor_scalar(out=arep[:], in0=arep[:], scalar1=-1.0, scalar2=None, op0=ALU.mult),
          r=["arep"], w=["arep"])
    S.add("dve", lambda h: h.tensor_tensor(out=cdiag[:], in0=ident_f[:, None, :].to_broadcast([128, 56, 128]),
                                           in1=convw[:, :].unsqueeze(2).to_broadcast([128, 56, 128]), op=ALU.mult),
          r=[], w=["cdiag"])
    S.add("dve", lambda h: h.tensor_tensor(out=ddiag[:], in0=ident_f[:, None, :].to_broadcast([128, 12, 128]),
                                           in1=dskip[:, :].unsqueeze(2).to_broadcast([128, 12, 128]), op=ALU.mult),
          r=[], w=["ddiag"])
    for g in range(4):
        blk, half = g // 2, g % 2
        dma("sp", stage2[half * 64:(half + 1) * 64, blk * 128 + half * 64: blk * 128 + half * 64 + 64],
            wpool_d[g], [], [], "init2")
    S.barrier()
    S.add("dve", lambda h: h.tensor_copy(out=wpool_b[:], in_=stage2[:, 0:256].rearrange("p (b c) -> p b c", b=2)),
          r=[], w=["wpool_b", "stage2"])
    S.add("act", lambda h: h.activation(out=cT[:], in_=cT[:], func=AF.Silu), r=[], w=["cT"])
    stg = [stage, stage2]
    stn = ["stage", "stage2"]
    psM = psW[:, 0:48 * nseq]
    wada_v = wada_d.rearrange("(kc p) n -> p kc n", p=128)
    for jg in range(12):
        si = jg % 2
        sg, rs = stg[si], stn[si]
        sgv = sg[:, 0:4096].rearrange("p (kc n) -> p kc n", n=512)
        dma("sp", sgv, wada_v[:, :, jg * 512:(jg + 1) * 512], [], [rs], "wst%d" % si)
        for jl in range(4):
            j = jg * 4 + jl
            for kc in range(KC):
                S.add("pe", lambda h, sgv=sgv, j=j, jl=jl, kc=kc: h.matmul(
                    psW[:, j * nseq:(j + 1) * nseq], lhsT=sgv[:, kc, jl * 128:(jl + 1) * 128], rhs=cT[:, kc, :],
                    start=(kc == 0), stop=(kc == KC - 1)), r=[rs, "cT"], w=["psW0"])
    S.add("dve", lambda h: h.tensor_tensor(out=modp[:], in0=psM.rearrange("p (j s) -> p j s", s=nseq),
                                           in1=badaT[:, :].unsqueeze(2).to_broadcast([128, 48, nseq]), op=ALU.add),
          r=["psW0"], w=["modp"])
    for j0 in (8, 32):
        S.add("dve", lambda h, j0=j0: h.tensor_scalar(out=modp[:, j0:j0 + 8, :], in0=modp[:, j0:j0 + 8, :],
                                                       scalar1=1.0, scalar2=None, op0=ALU.add),
              r=["modp"], w=["modp"])
    cast_engs = ["dve", "pool", "act"]
    ci = 0
    for kc in range(KC):
        si = kc % 2
        sg, rs = stg[si], stn[si]
        dma("sp", sg[:, 0:INP], win_d[kc * 128:(kc + 1) * 128, :], [], [rs], "wst%d" % si)
        for (a, b) in ((0, 1024), (1024, 2048), (2048, INP)):
            eng = cast_engs[ci % 3]
            ci += 1
            if eng == "act":
                S.add("act", lambda h, sg=sg, kc=kc, a=a, b=b: h.activation(out=win_b[:, kc, a:b], in_=sg[:, a:b],
                                                                            func=AF.Copy), r=[rs], w=[])
            else:
                S.add(eng, lambda h, sg=sg, kc=kc, a=a, b=b: h.tensor_copy(out=win_b[:, kc, a:b], in_=sg[:, a:b]),
                      r=[rs], w=[])
    for kc in range(KC):
        si = kc % 2
        sg, rs = stg[si], stn[si]
        dma("sp", sg[:, 0:D], wout_d[kc * 128:(kc + 1) * 128, :], [], [rs], "wst%d" % si)
        eng = cast_engs[kc % 3]
        if eng == "act":
            S.add("act", lambda h, sg=sg, kc=kc: h.activation(out=wout_b[:, kc, :], in_=sg[:, 0:D], func=AF.Copy),
                  r=[rs], w=[])
        else:
            S.add(eng, lambda h, sg=sg, kc=kc: h.tensor_copy(out=wout_b[:, kc, :], in_=sg[:, 0:D]), r=[rs], w=[])
    S.barrier()
    if "I" in debug:
        dump("modp", modp[:], [128, 48, nseq], [])
        dump("ident", ident_f[:], [128, 128], [])
        dump("mJL", mJL[:], [128, 128], [])
        return finish()
    stI.close()

    repA = sba("repA", [128, 3, D])
    dg = sba("dg", [128, 4, 128])
    xt = [sba("xt%d" % i, [128, D]) for i in range(2)]
    junk = sba("junk", [128, D], BF16)
    xn = sba("xn", [128, D], BF16)
    hT = sba("hT", [128, KC, TM], BF16)
    ubuf = sba("ubuf", [128, 2, 16 + TM])
    sA = sba("sA", [128, 2, 16 + TM])
    sB = sba("sB", [128, 2, 16 + TM])
    pmean = sba("pmean", [128, 2, TM])
    pdiff = sba("pdiff", [128, 2, TM], BF16)
    xpre = sba("xpre", [128, 14, 4 + TM], BF16)
    xact = sba("xact", [128, 14, TM], BF16)
    mixT = sba("mixT", [128, KC, TM], BF16)
    ctmp = sba("ctmp", [128, 2, 16])
    ctmp2 = sba("ctmp2", [128, 14, 4])
    sz = sba("sz", [128, 768])
    sm = sba("sm", [128, 8, 12])
    cum = sba("cum", [128, 24])
    e24 = sba("e24", [128, 24])
    rhsda = [sba("rhsda%d" % i, [128, 3, 128]) for i in range(2)]
    xc = sba("xc", [128, 768], BF16)
    xdte = sba("xdte", [128, 768], BF16)
    xtm = sba("xtm", [128, 768], BF16)
    btm = sba("btm", [128, 512], BF16)
    cbm = [sba("cbm%d" % i, [128, 128]) for i in range(2)]
    mt = [sba("mt%d" % i, [128, 3, 128], BF16) for i in range(2)]
    ytmp = [sba("ytmp%d" % i, [128, 192]) for i in range(2)]
    ych = sba("ych", [128, 768])
    ygb = sba("ygb", [128, 768], BF16)
    Sst = sba("Sst", [128, 4, 192])
    Sbf = sba("Sbf", [128, 4, 192], BF16)
    x1 = sba("x1", [128, D])
    xn2 = sba("xn2", [128, D])
    h2t = sba("h2t", [128, D])
    h2b = sba("h2b", [128, D], BF16)
    h2T = sba("h2T", [128, KC, 128])
    stat = sba("stat", [128, 16])
    rt = sba("rt", [128, 8, 36])
    abf = sba("abf", [128, 32], BF16)

    SQ, EXP, LN, SILU, COPY = AF.Square, AF.Exp, AF.Ln, AF.Silu, AF.Copy

    def rstd_ops(ss_ap, out_ap, n, res_in, res_out):
        S.add("dve", lambda h: h.tensor_scalar(out=out_ap, in0=ss_ap, scalar1=1.0 / n, scalar2=EPS,
                                               op0=ALU.mult, op1=ALU.add), r=[res_in], w=[res_out])
        S.add("act", lambda h: h.activation(out=out_ap, in_=out_ap, func=LN), r=[res_out], w=[res_out])
        S.add("act", lambda h: h.activation(out=out_ap, in_=out_ap, func=EXP, scale=-0.5), r=[res_out], w=[res_out])

    def rep_rows(dst_fn, j0, sidx, res_w):
        for q in range(2):
            S.add("dve", lambda h, q=q: h.tensor_tensor(
                out=dg[:], in0=ident_f[:, None, :].to_broadcast([128, 4, 128]),
                in1=modp[:, j0 + 4 * q:j0 + 4 * q + 4, sidx:sidx + 1].to_broadcast([128, 4, 128]), op=ALU.mult),
                r=["modp"], w=["dg"])
            S.add("pe", lambda h: h.matmul(psW[:, 0:512], lhsT=ones_f[:], rhs=dg[:].rearrange("p a b -> p (a b)"),
                                           start=True, stop=True), r=["dg"], w=["psW0"])
            S.add("act", lambda h, q=q: h.activation(out=dst_fn(q), in_=psW[:, 0:512], func=COPY),
                  r=["psW0"], w=[res_w])

    XPRE_ALL = ["xpre%d" % b for b in range(14)]
    tile_i = 0
    for s in range(nseq):
        for gi, j0 in enumerate((16, 24, 32)):
            rep_rows(lambda q, gi=gi: repA[:, gi, q * 512:(q + 1) * 512], j0, s, "repA")
        S.add("dve", lambda h: h.memset(ubuf[:, :, 0:16], 0.0), w=["ubuf"])
        S.add("dve", lambda h: h.memset(xpre[:, :, 0:4], 0.0), w=XPRE_ALL)
        S.add("pool", lambda h: h.memset(Sst[:], 0.0), w=["Sst%d" % g_ for g_ in range(4)])
        S.add("dve", lambda h: h.memset(Sbf[:], 0.0), w=["Sbf%d" % g_ for g_ in range(4)])

        for m in range(NMT):
            tok0 = s * seqlen + m * TM
            if "A0" in debug:
                S.barrier()
                dump("repA", repA[:], [128, 3, D], [])
                return finish()
            for c in range(CPM):
                ti = tile_i + c
                xs_ = xt[ti % 2]
                xr = "xt%d" % (ti % 2)
                dma("sp", xs_[:], x_d[tok0 + c * 128: tok0 + (c + 1) * 128, :], [], [xr], "xl%d" % (ti % 2))
                S.add("act", lambda h, xs_=xs_: h.activation(out=junk[:], in_=xs_[:], func=SQ, accum_out=stat[:, 0:1]),
                      r=[xr, "junk"], w=["junk", "stat0"])
                rstd_ops(stat[:, 0:1], stat[:, 1:2], D, "stat0", "stat1")
                S.add("act", lambda h, xs_=xs_: h.activation(out=xn[:], in_=xs_[:], func=COPY, scale=stat[:, 1:2]),
                      r=[xr, "stat1"], w=["xn"])
                for kc in range(KC):
                    S.add("pe", lambda h, kc=kc: h.transpose(psT_bf[:, kc * 128:(kc + 1) * 128],
                                                             xn[:, kc * 128:(kc + 1) * 128], ident_b[:]),
                          r=["xn"], w=["psT"])
                for kc in range(KC):
                    S.add("dve", lambda h, kc=kc, c=c: h.tensor_scalar(
                        out=hT[:, kc, c * 128:(c + 1) * 128], in0=psT_bf[:, kc * 128:(kc + 1) * 128],
                        scalar1=modp[:, 8 + kc, s:s + 1], scalar2=modp[:, kc, s:s + 1], op0=ALU.mult, op1=ALU.add),
                        r=["psT"], w=["hT"])
            if "A1" in debug:
                S.barrier()
                dump("hT", hT[:], [128, KC, TM], [], BF16)
                return finish()
            for blk in range(16):
                col0 = C_U + blk * 128
                bank = blk % 2
                pw = psW[:, bank * 512:bank * 512 + TM]
                for kc in range(KC):
                    S.add("pe", lambda h, kc=kc, col0=col0, pw=pw: h.matmul(
                        pw, lhsT=win_b[:, kc, col0:col0 + 128], rhs=hT[:, kc, :], start=(kc == 0), stop=(kc == KC - 1)),
                        r=["hT"], w=["psW%d" % bank])
                if blk < 2:
                    S.add("act", lambda h, blk=blk, pw=pw: h.activation(out=ubuf[:, blk, 16:16 + TM], in_=pw, func=COPY),
                          r=["psW%d" % bank], w=["ubuf"])
                elif blk % 2:
                    S.add("dve", lambda h, blk=blk, pw=pw: h.tensor_copy(out=xpre[:, blk - 2, 4:4 + TM], in_=pw),
                          r=["psW%d" % bank], w=["xpre%d" % (blk - 2)])
                else:
                    S.add("act", lambda h, blk=blk, pw=pw: h.activation(out=xpre[:, blk - 2, 4:4 + TM], in_=pw, func=COPY),
                          r=["psW%d" % bank], w=["xpre%d" % (blk - 2)])
            if "A15" in debug:
                S.barrier()
                dump("ubuf", ubuf[:, :, 16:16 + TM], [128, 2, TM], [])
                dump("xpre", xpre[:, :, 4:4 + TM], [128, 14, TM], [], BF16)
                return finish()
            E = 16 + TM
            S.add("pool", lambda h: h.tensor_tensor(out=sA[:, :, 1:E], in0=ubuf[:, :, 1:E], in1=ubuf[:, :, 0:E - 1], op=ALU.add),
                  r=["ubuf"], w=["sA"])
            for blk in range(2):
                S.add("dve", lambda h, blk=blk: h.tensor_scalar(out=pmean[:, blk, :], in0=sA[:, blk, 16:E],
                                                                 scalar1=pcoef[:, blk, 0:1], scalar2=None, op0=ALU.mult),
                      r=["sA"], w=["pmean"])
            prev, prevn = sA, "sA"
            for wi, sh in ((1, 2), (2, 4), (3, 8)):
                cur, curn = (sB, "sB") if prev is sA else (sA, "sA")
                lo = 2 * sh - 1
                S.add("pool", lambda h, cur=cur, prev=prev, lo=lo, sh=sh: h.tensor_tensor(
                    out=cur[:, :, lo:E], in0=prev[:, :, lo:E], in1=prev[:, :, lo - sh:E - sh], op=ALU.add),
                    r=[prevn], w=[curn])
                for blk in range(2):
                    S.add("dve", lambda h, cur=cur, wi=wi, blk=blk: h.scalar_tensor_tensor(
                        out=pmean[:, blk, :], in0=cur[:, blk, 16:E], scalar=pcoef[:, blk, wi:wi + 1],
                        in1=pmean[:, blk, :], op0=ALU.mult, op1=ALU.add), r=[curn, "pmean"], w=["pmean"])
                prev, prevn = cur, curn
            if m == 0:
                S.add("dve", lambda h: h.tensor_tensor(out=pmean[:, :, 0:16], in0=pmean[:, :, 0:16], in1=pratio[:], op=ALU.mult),
                      r=["pmean"], w=["pmean"])
            S.add("dve", lambda h: h.tensor_tensor(out=pdiff[:], in0=pmean[:], in1=ubuf[:, :, 16:E], op=ALU.subtract),
                  r=["pmean", "ubuf"], w=["pdiff"])
            S.add("dve", lambda h: h.tensor_copy(out=ctmp[:], in_=ubuf[:, :, TM:E]), r=["ubuf"], w=["ctmp"])
            S.add("dve", lambda h: h.tensor_copy(out=ubuf[:, :, 0:16], in_=ctmp[:]), r=["ctmp"], w=["ubuf"])
            def pool_mm():
                for blk in range(2):
                    pw = psW[:, blk * 512:blk * 512 + TM]
                    S.add("pe", lambda h, blk=blk, pw=pw: h.matmul(pw, lhsT=wpool_b[:, blk, :], rhs=pdiff[:, blk, :],
                                                                   start=True, stop=True), r=["pdiff"], w=["psW%d" % blk])
                    S.add("act", lambda h, blk=blk, pw=pw: h.activation(out=mixT[:, blk, :], in_=pw, func=COPY,
                                                                        scale=pscale[:, blk:blk + 1]),
                          r=["psW%d" % blk], w=["mixT_p"])

            if "A17" in debug:
                S.barrier()
                dump("mixTp", mixT[:, 0:2, :], [128, 2, TM], [], BF16)
                return finish()
            for blk in range(14):
                bank = blk % 2
                pw = psW[:, bank * 512:bank * 512 + TM]
                for k in range(4):
                    S.add("pe", lambda h, blk=blk, k=k, pw=pw: h.matmul(
                        pw, lhsT=cdiag[:, blk * 4 + k, :], rhs=xpre[:, blk, 1 + k:1 + k + TM], start=(k == 0), stop=(k == 3)),
                        r=["xpre%d" % blk], w=["psW%d" % bank])
                S.add("act", lambda h, blk=blk, pw=pw: h.activation(out=xact[:, blk, :], in_=pw, func=SILU,
                                                                    bias=convb[:, blk:blk + 1]),
                      r=["psW%d" % bank], w=["xact"])
            if "A18" in debug:
                S.barrier()
                dump("xact", xact[:], [128, 14, TM], [], BF16)
                return finish()
            S.add("dve", lambda h: h.tensor_copy(out=ctmp2[:], in_=xpre[:, :, TM:TM + 4]), r=XPRE_ALL, w=["ctmp2"])
            S.add("dve", lambda h: h.tensor_copy(out=xpre[:, :, 0:4], in_=ctmp2[:]), r=["ctmp2"], w=XPRE_ALL)

            if "A2" in debug:
                S.barrier()
                dump("hT", hT[:], [128, KC, TM], [], BF16)
                dump("xact", xact[:], [128, 14, TM], [], BF16)
                dump("mixTp", mixT[:, 0:2, :], [128, 2, TM], [], BF16)
                return finish()
            for c in range(CPM):
                ti = tile_i + c
                cs = slice(c * 128, (c + 1) * 128)
                xs_ = xt[ti % 2]
                xr = "xt%d" % (ti % 2)
                for (o0, o1, w0) in ((0, 512, C_Z), (512, 768, C_Z + 512)):
                    bank = o0 // 512
                    for kc in range(KC):
                        S.add("pe", lambda h, kc=kc, o0=o0, o1=o1, w0=w0: h.matmul(
                            psW[:, o0:o1], lhsT=hT[:, kc, cs], rhs=win_b[:, kc, w0:w0 + (o1 - o0)],
                            start=(kc == 0), stop=(kc == KC - 1)), r=["hT"], w=["psW%d" % bank])
                for kc in range(KC):
                    S.add("pe", lambda h, kc=kc: h.matmul(psS[:, 24:36], lhsT=hT[:, kc, cs], rhs=win_b[:, kc, C_DT:C_DT + 12],
                                                           start=(kc == 0), stop=(kc == KC - 1)), r=["hT"], w=["psS"])
                S.add("act", lambda h: h.activation(out=sz[:], in_=psW[:, 0:768], func=SILU), r=["psW0", "psW1"], w=["sz"])
                v, aa, ee, dtc, da, dte, dtdte = (sm[:, i, :] for i in range(7))
                S.add("dve", lambda h: h.tensor_tensor(out=v, in0=psS[:, 24:36], in1=dtb[:], op=ALU.add), r=["psS"], w=["sm0"])
                S.add("dve", lambda h: h.tensor_scalar(out=aa, in0=v, scalar1=0.0, scalar2=-2.0, op0=ALU.max, op1=ALU.mult), r=["sm0"], w=["sm1"])
                S.add("dve", lambda h: h.tensor_tensor(out=aa, in0=aa, in1=v, op=ALU.add), r=["sm0", "sm1"], w=["sm1"])
                S.add("act", lambda h: h.activation(out=ee, in_=aa, func=EXP), r=["sm1"], w=["sm2"])
                S.add("act", lambda h: h.activation(out=ee, in_=ee, func=LN, bias=1.0), r=["sm2"], w=["sm2"])
                S.add("dve", lambda h: h.scalar_tensor_tensor(out=dtc, in0=v, scalar=0.0, in1=ee, op0=ALU.max, op1=ALU.add),
                      r=["sm0", "sm2"], w=["sm3"])
                S.add("dve", lambda h: h.tensor_tensor(out=da, in0=dtc, in1=arep[:], op=ALU.mult), r=["sm3"], w=["sm4"])
                S.add("pe", lambda h: h.matmul(psS[:, 0:12], lhsT=mJL[:], rhs=da, start=True, stop=True), r=["sm4"], w=["psS"])
                S.add("pe", lambda h: h.matmul(psS[:, 12:24], lhsT=ones_f[:], rhs=da, start=True, stop=True), r=["sm4"], w=["psS"])
                S.add("dve", lambda h: h.tensor_copy(out=cum[:], in_=psS[:, 0:24]), r=["psS"], w=["cum"])
                S.add("act", lambda h: h.activation(out=e24[:], in_=cum[:], func=EXP), r=["cum"], w=["e24"])
                S.add("dve", lambda h: h.tensor_tensor(out=dte, in0=cum[:, 12:24], in1=cum[:, 0:12], op=ALU.subtract), r=["cum"], w=["sm5"])
                S.add("act", lambda h: h.activation(out=dte, in_=dte, func=EXP), r=["sm5"], w=["sm5"])
                S.add("dve", lambda h: h.tensor_tensor(out=dtdte, in0=dte, in1=dtc, op=ALU.mult), r=["sm5", "sm3"], w=["sm6"])
                if c == 0 and "C1" in debug:
                    S.barrier()
                    dump('sm', sm[:], [128, 8, 12], [])
                    dump('e24', e24[:], [128, 24], [])
                    dump('sz', sz[:], [128, 768], [])
                    return finish()
                for b6 in range(6):
                    S.add("pe", lambda h, b6=b6: h.transpose(psT_bf[:, b6 * 128:(b6 + 1) * 128], xact[:, b6, cs], ident_b[:]),
                          r=["xact"], w=["psT"])
                psB = psW[:, 768:1024].bitcast(BF16)
                for g in range(4):
                    S.add("pe", lambda h, g=g: h.transpose(psB[:, g * 128:(g + 1) * 128], xact[:, 6 + g, cs], ident_b[:]),
                          r=["xact"], w=["psW1"])
                pxv = psT_bf[:, 0:768].rearrange("p (a b) -> p a b", b=64)
                S.add("dve", lambda h: h.tensor_tensor(out=xc[:].rearrange("p (a b) -> p a b", b=64), in0=pxv,
                                                       in1=dtc.unsqueeze(2).to_broadcast([128, 12, 64]), op=ALU.mult),
                      r=["psT", "sm3"], w=["xc"])
                S.add("dve", lambda h: h.tensor_tensor(out=xdte[:].rearrange("p (a b) -> p a b", b=64), in0=pxv,
                                                       in1=dtdte.unsqueeze(2).to_broadcast([128, 12, 64]), op=ALU.mult),
                      r=["psT", "sm6"], w=["xdte"])
                S.add("dve", lambda h: h.tensor_scalar(out=xtm[:], in0=psT_bf[:, 0:768], scalar1=1.0, scalar2=None, op0=ALU.mult),
                      r=["psT"], w=["xtm"])
                S.add("dve", lambda h: h.tensor_scalar(out=btm[:], in0=psB, scalar1=1.0, scalar2=None, op0=ALU.mult),
                      r=["psW1"], w=["btm"])
                if c == 0 and "C2" in debug:
                    S.barrier()
                    dump('xc', xc[:], [128, 768], [], BF16)
                    dump('btm', btm[:], [128, 512], [], BF16)
                    return finish()
                def stage_p(g):
                    gb = g % 2
                    sg_ = psSG[:, gb * 512:(gb + 1) * 512]
                    sgr = "psSG%d" % gb
                    S.add("pool", lambda h: h.affine_select(
                        out=rhsda[gb][:], in_=sm[:, 4, 3 * g:3 * g + 3].unsqueeze(2).to_broadcast([128, 3, 128]),
                        pattern=[[0, 3], [1, 128]], compare_op=ALU.is_ge, fill=0.0, base=0, channel_multiplier=-1),
                        r=["sm4"], w=["rhsda%d" % gb])
                    S.add("pe", lambda h: h.matmul(sg_[:, 0:384], lhsT=mST[:], rhs=rhsda[gb][:].rearrange("p a b -> p (a b)"),
                                                   start=True, stop=True), r=["rhsda%d" % gb], w=[sgr])
                    S.add("pe", lambda h: h.matmul(sg_[:, 384:512], lhsT=xact[:, 6 + g, cs], rhs=xact[:, 10 + g, cs],
                                                   start=True, stop=True), r=["xact"], w=[sgr])
                    S.add("act", lambda h: h.activation(out=sg_[:, 0:384], in_=sg_[:, 0:384], func=EXP), r=[sgr], w=[sgr])
                    S.add("dve", lambda h: h.tensor_tensor(out=cbm[gb][:], in0=sg_[:, 384:512], in1=mJL[:], op=ALU.mult),
                          r=[sgr], w=["cbm%d" % gb])
                    S.add("dve", lambda h: h.tensor_tensor(
                        out=mt[gb][:], in0=sg_[:, 0:384].rearrange("p (a b) -> p a b", b=128),
                        in1=cbm[gb][:, None, :].to_broadcast([128, 3, 128]), op=ALU.mult),
                        r=[sgr, "cbm%d" % gb], w=["mt%d" % gb])

                def stage_q(g):
                    gb = g % 2
                    py = psY[:, gb * 512:(gb + 1) * 512]
                    pyr = "psY%d" % gb
                    for r_ in range(3):
                        hh = 3 * g + r_
                        S.add("pe", lambda h: h.matmul(
                            py[:, r_ * 64:(r_ + 1) * 64], lhsT=mt[gb][:, r_, :], rhs=xc[:, hh * 64:(hh + 1) * 64],
                            start=True, stop=False), r=["mt%d" % gb, "xc"], w=[pyr])
                        S.add("pe", lambda h: h.matmul(
                            py[:, r_ * 64:(r_ + 1) * 64], lhsT=ddiag[:, hh, :], rhs=xtm[:, hh * 64:(hh + 1) * 64],
                            start=False, stop=True), r=["xtm"], w=[pyr])
                    S.add("pe", lambda h: h.matmul(py[:, 192:384], lhsT=xact[:, 10 + g, cs], rhs=Sbf[:, g, :],
                                                   start=True, stop=True), r=["xact", "Sbf%d" % g], w=[pyr])
                    S.add("pe", lambda h: h.matmul(psS[:, 192:384], lhsT=btm[:, g * 128:(g + 1) * 128],
                                                   rhs=xdte[:, g * 192:(g + 1) * 192], start=True, stop=True),
                          r=["btm", "xdte"], w=["psS"])
                    S.add("dve", lambda h: h.tensor_tensor(
                        out=ytmp[gb][:].rearrange("p (a b) -> p a b", b=64), in0=py[:, 192:384].rearrange("p (a b) -> p a b", b=64),
                        in1=e24[:, 3 * g:3 * g + 3].unsqueeze(2).to_broadcast([128, 3, 64]), op=ALU.mult),
                        r=[pyr, "e24"], w=["ytmp%d" % gb])
                    S.add("dve", lambda h: h.tensor_tensor(out=ych[:, g * 192:(g + 1) * 192], in0=py[:, 0:192],
                                                           in1=ytmp[gb][:], op=ALU.add), r=[pyr, "ytmp%d" % gb], w=["ych%d" % g])
                    S.add("pool", lambda h: h.tensor_tensor(
                        out=Sst[:, g, :].rearrange("p (a b) -> p a b", b=64), in0=Sst[:, g, :].rearrange("p (a b) -> p a b", b=64),
                        in1=e24[:, 12 + 3 * g:15 + 3 * g].unsqueeze(2).to_broadcast([128, 3, 64]), op=ALU.mult),
                        r=["Sst%d" % g, "e24"], w=["Sst%d" % g])
                    S.add("dve", lambda h: h.tensor_tensor(out=Sst[:, g, :], in0=psS[:, 192:384], in1=Sst[:, g, :], op=ALU.add),
                          r=["psS", "Sst%d" % g], w=["Sst%d" % g])
                    S.add("pool", lambda h: h.tensor_copy(out=Sbf[:, g, :], in_=Sst[:, g, :]), r=["Sst%d" % g], w=["Sbf%d" % g])

                stage_p(0)
                stage_p(1)
                stage_q(0)
                stage_p(2)
                stage_q(1)
                stage_p(3)
                stage_q(2)
                stage_q(3)
                if c == 0 and "C3" in debug:
                    S.barrier()
                    dump('ych', ych[:], [128, 768], [])
                    return finish()
                S.add("pool", lambda h: h.tensor_tensor(out=ych[:], in0=ych[:], in1=sz[:], op=ALU.mult), r=["ych0", "ych1", "ych2", "ych3"] + ["sz"], w=["ych"])
                for g in range(4):
                    S.add("act", lambda h, g=g: h.activation(out=junk[:, 0:192], in_=ych[:, g * 192:(g + 1) * 192], func=SQ,
                                                             accum_out=stat[:, 4 + g:5 + g]), r=["ych", "ych0", "ych1", "ych2", "ych3", "junk"], w=["junk", "stat4"])
                rstd_ops(stat[:, 4:8], stat[:, 8:12], 192, "stat4", "stat8")
                S.add("dve", lambda h: h.tensor_tensor(out=ygb[:].rearrange("p (a b) -> p a b", b=192),
                                                       in0=ych[:].rearrange("p (a b) -> p a b", b=192),
                                                       in1=stat[:, 8:12].unsqueeze(2).to_broadcast([128, 4, 192]), op=ALU.mult),
                      r=["ych", "ych0", "ych1", "ych2", "ych3", "stat8"], w=["ygb"])
                for b6 in range(6):
                    S.add("pe", lambda h, b6=b6: h.transpose(psT_bf[:, b6 * 128:(b6 + 1) * 128], ygb[:, b6 * 128:(b6 + 1) * 128], ident_b[:]),
                          r=["ygb"], w=["psT"])
                S.add("dve", lambda h: h.tensor_tensor(out=mixT[:, 2:8, cs], in0=psT_bf[:, 0:768].rearrange("p (a b) -> p a b", b=128),
                                                       in1=normw[:, :].unsqueeze(2).to_broadcast([128, 6, 128]), op=ALU.mult),
                      r=["psT"], w=["mixT_s"])
                if c == 0 and "C4" in debug:
                    S.barrier()
                    dump('mixTs', mixT[:, 2:8, 0:128], [128, 6, 128], [], BF16)
                    return finish()
                if c == 0:
                    pool_mm()
                for half in range(2):
                    for kc in range(KC):
                        S.add("pe", lambda h, half=half, kc=kc: h.matmul(
                            psW[:, half * 512:(half + 1) * 512], lhsT=mixT[:, kc, cs], rhs=wout_b[:, kc, half * 512:(half + 1) * 512],
                            start=(kc == 0), stop=(kc == KC - 1)), r=["mixT_s", "mixT_p"], w=["psW%d" % half])
                S.add("dve", lambda h: h.tensor_tensor(out=x1[:], in0=psW[:, :], in1=repA[:, 0, :], op=ALU.mult),
                      r=["psW0", "psW1", "repA"], w=["x1"])
                S.add("pool", lambda h, xs_=xs_: h.tensor_tensor(out=x1[:], in0=x1[:], in1=xs_[:], op=ALU.add),
                      r=["x1", xr], w=["x1"])
                row0 = tok0 + c * 128
                dma("sp", x1s_d[row0:row0 + 128, :], x1[:], ["x1"], [], "x1st")
                if c == 0 and "C5" in debug:
                    S.barrier()
                    dump('x1', x1[:], [128, D], [])
                    return finish()
                S.add("act", lambda h: h.activation(out=junk[:], in_=x1[:], func=SQ, accum_out=stat[:, 2:3]),
                      r=["x1", "junk"], w=["junk", "stat2"])
                rstd_ops(stat[:, 2:3], stat[:, 3:4], D, "stat2", "stat3")
                S.add("act", lambda h: h.activation(out=xn2[:], in_=x1[:], func=COPY, scale=stat[:, 3:4]),
                      r=["x1", "stat3"], w=["xn2"])
                S.add("pool", lambda h: h.tensor_tensor(out=h2t[:], in0=xn2[:], in1=repA[:, 2, :], op=ALU.mult), r=["xn2", "repA"], w=["h2t"])
                S.add("pool", lambda h: h.tensor_tensor(out=h2b[:], in0=h2t[:], in1=repA[:, 1, :], op=ALU.add),
                      r=["h2t", "repA"], w=["h2b"])
                dma("sp", h2s_d[row0:row0 + 128, :], h2b[:], ["h2b"], [], "h2st")
                for kc in range(KC):
                    S.add("pe", lambda h, kc=kc: h.transpose(psSG[:, kc * 128:(kc + 1) * 128], xn2[:, kc * 128:(kc + 1) * 128], ident_f[:]),
                          r=["xn2"], w=["psSG%d" % (kc // 4)])
                for kc in range(KC):
                    S.add("dve", lambda h, kc=kc: h.tensor_scalar(
                        out=h2T[:, kc, :], in0=psSG[:, kc * 128:(kc + 1) * 128], scalar1=modp[:, 32 + kc, s:s + 1],
                        scalar2=modp[:, 24 + kc, s:s + 1], op0=ALU.mult, op1=ALU.add),
                        r=["psSG%d" % (kc // 4)], w=["h2T"])
                for kc in range(KC):
                    S.add("pe", lambda h, kc=kc: h.matmul(psS[:, 40:76], lhsT=h2T[:, kc, :], rhs=wgr[:, kc, :],
                                                           start=(kc == 0), stop=(kc == KC - 1)), r=["h2T"], w=["psS"])
                if c == 0 and "C6" in debug:
                    S.barrier()
                    dump('h2b', h2b[:], [128, D], [], BF16)
                    dump('h2T', h2T[:], [128, KC, 128], [])
                    return finish()
                lg = rt[:, 0, :]
                me = rt[:, 1, 0:32]
                me2 = rt[:, 2, 0:32]
                sc_ = rt[:, 3, :]
                goh = rt[:, 4, 0:4]
                pen = rt[:, 4, 4:8]
                gs = rt[:, 4, 8:12]
                oh1 = rt[:, 5, 0:32]
                oh2 = rt[:, 6, 0:32]
                tm_ = rt[:, 7, 0:32]
                V = "dve"
                RT = ["rt"]
                S.add(V, lambda h: h.tensor_tensor(out=lg, in0=psS[:, 40:76], in1=bgr[:], op=ALU.add), r=["psS"], w=RT)
                S.add(V, lambda h: h.tensor_reduce(out=sc_[:, 0:1], in_=lg[:, 0:4], axis=AX.X, op=ALU.max), r=RT, w=RT)
                S.add(V, lambda h: h.tensor_scalar(out=gs, in0=lg[:, 0:4], scalar1=sc_[:, 0:1], scalar2=None, op0=ALU.subtract), r=RT, w=RT)
                S.add("act", lambda h: h.activation(out=gs, in_=gs, func=EXP, accum_out=sc_[:, 1:2]), r=RT, w=RT)
                S.add(V, lambda h: h.reciprocal(out=sc_[:, 2:3], in_=sc_[:, 1:2]), r=RT, w=RT)
                S.add(V, lambda h: h.tensor_scalar(out=goh, in0=lg[:, 0:4], scalar1=sc_[:, 0:1], scalar2=None, op0=ALU.is_equal), r=RT, w=RT)
                S.add(V, lambda h: h.tensor_scalar(out=pen, in0=goh, scalar1=BIG, scalar2=-BIG, op0=ALU.mult, op1=ALU.add), r=RT, w=RT)
                S.add(V, lambda h: h.tensor_tensor(out=me.rearrange("p (a b) -> p a b", b=8), in0=lg[:, 4:36].rearrange("p (a b) -> p a b", b=8),
                                                   in1=pen.unsqueeze(2).to_broadcast([128, 4, 8]), op=ALU.add), r=RT, w=RT)
                S.add(V, lambda h: h.tensor_reduce(out=sc_[:, 3:4], in_=me, axis=AX.X, op=ALU.max), r=RT, w=RT)
                S.add(V, lambda h: h.tensor_scalar(out=oh1, in0=me, scalar1=sc_[:, 3:4], scalar2=None, op0=ALU.is_equal), r=RT, w=RT)
                S.add(V, lambda h: h.scalar_tensor_tensor(out=me2, in0=oh1, scalar=-BIG, in1=me, op0=ALU.mult, op1=ALU.add), r=RT, w=RT)
                S.add(V, lambda h: h.tensor_reduce(out=sc_[:, 4:5], in_=me2, axis=AX.X, op=ALU.max), r=RT, w=RT)
                S.add(V, lambda h: h.tensor_scalar(out=oh2, in0=me2, scalar1=sc_[:, 4:5], scalar2=None, op0=ALU.is_equal), r=RT, w=RT)
                S.add(V, lambda h: h.tensor_tensor(out=sc_[:, 5:6], in0=sc_[:, 4:5], in1=sc_[:, 3:4], op=ALU.subtract), r=RT, w=RT)
                S.add("act", lambda h: h.activation(out=sc_[:, 6:7], in_=sc_[:, 5:6], func=EXP), r=RT, w=RT)
                S.add(V, lambda h: h.tensor_scalar(out=sc_[:, 7:8], in0=sc_[:, 6:7], scalar1=1.0, scalar2=None, op0=ALU.add), r=RT, w=RT)
                S.add(V, lambda h: h.reciprocal(out=sc_[:, 8:9], in_=sc_[:, 7:8]), r=RT, w=RT)
                S.add(V, lambda h, ti=ti: h.tensor_tensor(out=gates[:, ti, 0:1], in0=sc_[:, 8:9], in1=sc_[:, 2:3], op=ALU.mult), r=RT, w=["gates"])
                S.add(V, lambda h, ti=ti: h.tensor_tensor(out=gates[:, ti, 1:2], in0=gates[:, ti, 0:1], in1=sc_[:, 6:7], op=ALU.mult), r=RT + ["gates"], w=["gates"])
                S.add(V, lambda h: h.tensor_tensor(out=abf[:], in0=oh1, in1=oh2, op=ALU.add), r=RT, w=["abf"])
                S.add("pe", lambda h: h.matmul(psS[:, 76:108], lhsT=mLT_b[:], rhs=abf[:], start=True, stop=True), r=["abf"], w=["psS"])
                S.add("pe", lambda h: h.matmul(psS[:, 108:140], lhsT=ones_b[:], rhs=abf[:], start=True, stop=True), r=["abf"], w=["psS"])
                for k, oh in ((0, oh1), (1, oh2)):
                    S.add(V, lambda h, oh=oh: h.tensor_tensor(out=tm_, in0=oh, in1=iota_e[:], op=ALU.mult), r=RT, w=RT)
                    S.add(V, lambda h, k=k, ti=ti: h.tensor_reduce(out=eidx[:, ti, k:k + 1], in_=tm_, axis=AX.X, op=ALU.add), r=RT, w=["eidx"])
                S.add(V, lambda h: h.tensor_tensor(out=me, in0=psS[:, 76:108], in1=totrun[:], op=ALU.add), r=["psS", "totrun"] + RT, w=RT)
                for k, oh in ((0, oh1), (1, oh2)):
                    S.add(V, lambda h, oh=oh: h.tensor_tensor(out=tm_, in0=oh, in1=me, op=ALU.mult), r=RT, w=RT)
                    S.add(V, lambda h, k=k, ti=ti: h.tensor_reduce(out=posk[:, ti, k:k + 1], in_=tm_, axis=AX.X, op=ALU.add), r=RT, w=["posk"])
                S.add(V, lambda h: h.tensor_tensor(out=totrun[:], in0=psS[:, 108:140], in1=totrun[:], op=ALU.add), r=["psS", "totrun"], w=["totrun"])
            tile_i += CPM

    if "A" in debug:
        S.barrier()
        dump("modp", modp[:], [128, 48, nseq], [])
        dump("gates", gates[:], [128, NT, 2], [])
        dump("eidx", eidx[:], [128, NT, 2], [])
        dump("posk", posk[:], [128, NT, 2], [])
        dump("hT", hT[:], [128, KC, TM], [], BF16)
        dump("xact", xact[:], [128, 14, TM], [], BF16)
        dump("mixTs", mixT[:, 2:8, :], [128, 6, TM], [], BF16)
        dump("mixTp", mixT[:, 0:2, :], [128, 2, TM], [], BF16)
        dump("ych", ych[:], [128, 768], [])
        dump("x1", x1[:], [128, D], [])
        dump("e24", e24[:], [128, 24], [])
        dump("sz", sz[:], [128, 768], [])
        dump("h2b", h2b[:], [128, D], [], BF16)
        return finish()

    S.barrier()
    stA.close()
    stB = ExitStack()

    g2rep = sb("g2rep", [128, nseq, D], stack=stB)
    stB1 = ExitStack()
    curB = [stB1]

    def sbb(name, shape, dt=F32):
        return sb(name, shape, dt, stack=curB[0])

    thr = sbb("thr", [128, 128])
    bst = sbb("bst", [128, NB])
    cmpb = sbb("cmpb", [128, 32 * 128])
    cmp2 = sbb("cmp2", [128, NB * 32])
    ohb = sbb("ohb", [128, NT * 32])
    nbk = sbb("nbk", [128, 32])
    ca = sbb("ca", [128, 32])
    cb_ = sbb("cb_", [128, 32])
    pstart = sbb("pstart", [128, 32])
    blke = sbb("blke", [128, NB])
    widx_f = sbb("widx_f", [128, NB])
    eqf = sbb("eqf", [128, NB])
    dst_f = sbb("dst_f", [128, NT, 2])
    pst = sbb("pst", [128, NT])
    dg = sbb("dg2", [128, 4, 128])

    S.add("pool", lambda h: h.iota(thr[:], pattern=[[128, 128]], base=0, channel_multiplier=0,
                                   allow_small_or_imprecise_dtypes=True), w=["thr"])
    S.add("pool", lambda h: h.iota(bst[:], pattern=[[128, NB]], base=0, channel_multiplier=0,
                                   allow_small_or_imprecise_dtypes=True), w=["bst"])
    V = "dve"
    S.add(V, lambda h: h.tensor_tensor(out=cmpb[:].rearrange("p (a b) -> p a b", b=128),
                                       in0=totrun[:, :].unsqueeze(2).to_broadcast([128, 32, 128]),
                                       in1=thr[:, None, :].to_broadcast([128, 32, 128]), op=ALU.is_gt),
          r=["thr", "totrun"], w=["cmpb"])
    S.add(V, lambda h: h.tensor_reduce(out=nbk[:], in_=cmpb[:].rearrange("p (a b) -> p a b", b=128), axis=AX.X, op=ALU.add),
          r=["cmpb"], w=["nbk"])
    S.add(V, lambda h: h.tensor_scalar(out=nbk[:], in0=nbk[:], scalar1=128.0, scalar2=None, op0=ALU.mult), r=["nbk"], w=["nbk"])
    S.add(V, lambda h: h.tensor_copy(out=ca[:], in_=nbk[:]), r=["nbk"], w=["ca"])
    a_, b_ = ca, cb_
    an, bn = "ca", "cb_"
    for sh in (1, 2, 4, 8, 16):
        S.add(V, lambda h, a_=a_, b_=b_, sh=sh: h.tensor_tensor(out=b_[:, sh:32], in0=a_[:, sh:32], in1=a_[:, 0:32 - sh], op=ALU.add),
              r=[an], w=[bn])
        S.add(V, lambda h, a_=a_, b_=b_, sh=sh: h.tensor_copy(out=b_[:, 0:sh], in_=a_[:, 0:sh]), r=[an, bn], w=[bn])
        a_, b_, an, bn = b_, a_, bn, an
    pends, pn = a_, an
    S.add(V, lambda h: h.tensor_tensor(out=pstart[:], in0=pends[:], in1=nbk[:], op=ALU.subtract), r=[pn, "nbk"], w=["pstart"])
    S.add(V, lambda h: h.tensor_tensor(out=cmp2[:].rearrange("p (a b) -> p a b", b=32),
                                       in0=pends[:, None, :].to_broadcast([128, NB, 32]),
                                       in1=bst[:, :].unsqueeze(2).to_broadcast([128, NB, 32]), op=ALU.is_le),
          r=[pn, "bst"], w=["cmp2"])
    S.add(V, lambda h: h.tensor_reduce(out=blke[:], in_=cmp2[:].rearrange("p (a b) -> p a b", b=32), axis=AX.X, op=ALU.add),
          r=["cmp2"], w=["blke"])
    S.add(V, lambda h: h.tensor_scalar(out=blke[:], in0=blke[:], scalar1=31.0, scalar2=None, op0=ALU.min), r=["blke"], w=["blke"])
    S.add(V, lambda h: h.tensor_scalar(out=widx_f[:], in0=blke[:], scalar1=128.0, scalar2=iota_p[:, 0:1], op0=ALU.mult, op1=ALU.add),
          r=["blke"], w=["widx_f"])
    S.add(V, lambda h: h.tensor_tensor(out=eqf[:, 2:NB], in0=blke[:, 2:NB], in1=blke[:, 0:NB - 2], op=ALU.is_equal), r=["blke"], w=["eqf"])
    S.add(V, lambda h: h.memset(eqf[:, 0:2], 0.0), r=["eqf"], w=["eqf"])
    for nm, kk, dst in (("w13f", 4, widx13_i), ("w2f", 2, widx2_i)):
        wf = sbb(nm, [128, NB, kk])
        bk = sbb(nm + "b", [128, NB])
        S.add(V, lambda h, bk=bk, kk=kk: h.tensor_scalar(out=bk[:], in0=widx_f[:], scalar1=float(kk), scalar2=None, op0=ALU.mult),
              r=["widx_f"], w=[nm + "b"])
        S.add(V, lambda h, bk=bk: h.scalar_tensor_tensor(out=bk[:], in0=eqf[:], scalar=40000.0, in1=bk[:], op0=ALU.mult, op1=ALU.add),
              r=["eqf", nm + "b"], w=[nm + "b"])
        S.add(V, lambda h, bk=bk, wf=wf, kk=kk: h.tensor_tensor(out=wf[:], in0=bk[:, :].unsqueeze(2).to_broadcast([128, NB, kk]),
                                                                in1=iota_e[:, None, 0:kk].to_broadcast([128, NB, kk]), op=ALU.add),
              r=[nm + "b"], w=[nm])
        S.add(V, lambda h, wf=wf, dst=dst: h.tensor_copy(out=dst[:], in_=wf[:]), r=[nm], w=["widx_i"])
    for k in range(2):
        S.add(V, lambda h, k=k: h.tensor_tensor(out=ohb[:].rearrange("p (a b) -> p a b", b=32),
                                               in0=iota_e[:, None, :].to_broadcast([128, NT, 32]),
                                               in1=eidx[:, :, k:k + 1].to_broadcast([128, NT, 32]), op=ALU.is_equal),
              r=["eidx", "pst"], w=["ohb"])
        S.add(V, lambda h: h.tensor_tensor(out=ohb[:].rearrange("p (a b) -> p a b", b=32),
                                           in0=ohb[:].rearrange("p (a b) -> p a b", b=32),
                                           in1=pstart[:, None, :].to_broadcast([128, NT, 32]), op=ALU.mult),
              r=["ohb", "pstart"], w=["ohb"])
        S.add(V, lambda h: h.tensor_reduce(out=pst[:], in_=ohb[:].rearrange("p (a b) -> p a b", b=32), axis=AX.X, op=ALU.add),
              r=["ohb"], w=["pst"])
        S.add(V, lambda h, k=k: h.tensor_tensor(out=dst_f[:, :, k:k + 1], in0=posk[:, :, k:k + 1], in1=pst[:, :].unsqueeze(2), op=ALU.add),
              r=["pst", "posk"], w=["dst_f"])
    S.add(V, lambda h: h.tensor_copy(out=dest_i[:], in_=dst_f[:]), r=["dst_f"], w=["dest_i"])
    for s in range(nseq):
        for q in range(2):
            S.add("dve", lambda h, q=q, s=s: h.tensor_tensor(
                out=dg[:], in0=ident_f[:, None, :].to_broadcast([128, 4, 128]),
                in1=modp[:, 40 + 4 * q:44 + 4 * q, s:s + 1].to_broadcast([128, 4, 128]), op=ALU.mult),
                r=[], w=["dg2"])
            S.add("pe", lambda h: h.matmul(psW[:, 0:512], lhsT=ones_f[:], rhs=dg[:].rearrange("p a b -> p (a b)"),
                                           start=True, stop=True), r=["dg2"], w=["psW0"])
            S.add("act", lambda h, q=q, s=s: h.activation(out=g2rep[:, s, q * 512:(q + 1) * 512], in_=psW[:, 0:512], func=COPY),
                  r=["psW0"], w=["g2rep"])

    if "B" in debug:
        S.barrier()
        dump("widx", widx13_i[:], [128, NB, 4], [], I32)
        dump("dest", dest_i[:], [128, NT, 2], [], I32)
        dump("blke", blke[:], [128, NB], [])
        dump("pstart", pstart[:], [128, 32], [])
        dump("totrun", totrun[:], [128, 32], [])
        return finish()

    hb = [sbb("hb%d" % i, [128, D], BF16) for i in range(2)]
    zt = sbb("zt", [128, 8, D], BF16)
    S.add("pool", lambda h: h.memset(zt[:], 0.0), r=[], w=["zt"])
    b0 = 0
    while b0 < NB:
        nb_ = min(8, NB - b0)
        dma("sp", xs_d[b0 * 128:(b0 + nb_) * 128, :].rearrange("(n p) d -> p n d", p=128), zt[:, 0:nb_, :], ["zt"], [], "zfill")
        b0 += nb_
    S.barrier()
    for t in range(NT):
        sl = t % 2
        dma("sp", hb[sl][:], h2s_d[t * 128:(t + 1) * 128, :], [], ["hb%d" % sl], "hbl%d" % sl)
        for k in range(2):
            S.add("pool", lambda h, t=t, k=k, sl=sl: h.indirect_dma_start(
                out=xs_d[:, :], out_offset=bass.IndirectOffsetOnAxis(ap=dest_i[:, t, k:k + 1], axis=0),
                in_=hb[sl][:], in_offset=None), r=["hb%d" % sl, "dest_i"], w=[], dma="sc%d" % sl)
    S.barrier()

    stB1.close()
    curB[0] = stB
    W13 = [sbb("W13_%d" % i, [128, 8, 1024], BF16) for i in range(2)]
    W2 = [sbb("W2_%d" % i, [128, 4, 1024], BF16) for i in range(2)]
    xb = [sbb("xb%d" % i, [128, D], BF16) for i in range(2)]
    xT = sbb("xT", [128, 8, 128], BF16)
    sa = sbb("sa", [128, 512])
    actb = sbb("actb", [128, 512], BF16)
    actT = sbb("actT", [128, 4, 128], BF16)
    yb = [sbb("yb%d" % i, [128, D]) for i in range(2)]
    reg13 = nc.gpsimd.alloc_register("bc13")
    reg2 = nc.gpsimd.alloc_register("bc2")
    S.add("pool", lambda h: h.reg_mov(reg13, NE * 512 - 1), r=[], w=[])
    S.add("pool", lambda h: h.reg_mov(reg2, NE * 256 - 1), r=[], w=[])
    w13v = w13_d.rearrange("e (r two) n -> (e r) (two n)", two=2)
    w2v = w2_d.rearrange("e (r two) n -> (e r) (two n)", two=2)
    for b in range(NB):
        sl = b % 2
        for j in range(4):
            S.add("pool", lambda h, b=b, sl=sl, j=j: h.indirect_dma_start(
                out=W13[sl][:, 2 * j:2 * j + 2, :].rearrange("p a n -> p (a n)"), out_offset=None, in_=w13v,
                in_offset=bass.IndirectOffsetOnAxis(ap=widx13_i[:, b, j:j + 1], axis=0),
                bounds_check=reg13, oob_is_err=False), r=["widx_i"], w=["W13_%d_%d" % (sl, j)], dma="w13_%d" % sl)
        for j in range(2):
            S.add("pool", lambda h, b=b, sl=sl, j=j: h.indirect_dma_start(
                out=W2[sl][:, 2 * j:2 * j + 2, :].rearrange("p a n -> p (a n)"), out_offset=None, in_=w2v,
                in_offset=bass.IndirectOffsetOnAxis(ap=widx2_i[:, b, j:j + 1], axis=0),
                bounds_check=reg2, oob_is_err=False), r=["widx_i"], w=["W2_%d_%d" % (sl, j)], dma="w2_%d" % sl)
        dma("sp", xb[sl][:], xs_d[b * 128:(b + 1) * 128, :], [], ["xb%d" % sl], "xbl%d" % sl)
        for kc in range(8):
            S.add("pe", lambda h, kc=kc, sl=sl: h.transpose(psT_bf[:, kc * 128:(kc + 1) * 128], xb[sl][:, kc::8], ident_b[:]),
                  r=["xb%d" % sl], w=["psT"])
        S.add("dve", lambda h: h.tensor_scalar(out=xT[:].rearrange("p a b -> p (a b)"), in0=psT_bf[:, :], scalar1=1.0, scalar2=None, op0=ALU.mult),
              r=["psT"], w=["xT"])
        for half in range(2):
            for kc in range(8):
                S.add("pe", lambda h, half=half, kc=kc, sl=sl: h.matmul(
                    psW[:, half * 512:(half + 1) * 512], lhsT=xT[:, kc, :], rhs=W13[sl][:, kc, half * 512:(half + 1) * 512],
                    start=(kc == 0), stop=(kc == 7)), r=["xT"] + ["W13_%d_%d" % (sl, k_) for k_ in range(4)], w=["psW%d" % half])
        S.add("act", lambda h: h.activation(out=sa[:], in_=psW[:, 0:512], func=SILU), r=["psW0"], w=["sa"])
        S.add("dve", lambda h: h.tensor_tensor(out=actb[:], in0=psW[:, 512:1024], in1=sa[:], op=ALU.mult), r=["psW1", "sa"], w=["actb"])
        for fc in range(4):
            S.add("pe", lambda h, fc=fc: h.transpose(psT_bf[:, fc * 128:(fc + 1) * 128], actb[:, fc::4], ident_b[:]),
                  r=["actb"], w=["psT"])
        S.add("dve", lambda h: h.tensor_scalar(out=actT[:].rearrange("p a b -> p (a b)"), in0=psT_bf[:, 0:512], scalar1=1.0, scalar2=None, op0=ALU.mult),
              r=["psT"], w=["actT"])
        for half in range(2):
            for fc in range(4):
                S.add("pe", lambda h, half=half, fc=fc, sl=sl: h.matmul(
                    psY[:, half * 512:(half + 1) * 512], lhsT=actT[:, fc, :], rhs=W2[sl][:, fc, half * 512:(half + 1) * 512],
                    start=(fc == 0), stop=(fc == 3)), r=["actT"] + ["W2_%d_%d" % (sl, k_) for k_ in range(2)], w=["psY%d" % half])
        S.add("act", lambda h, sl=sl: h.activation(out=yb[sl][:, 0:512], in_=psY[:, 0:512], func=COPY), r=["psY0"], w=["yb%da" % sl])
        S.add("act", lambda h, sl=sl: h.activation(out=yb[sl][:, 512:1024], in_=psY[:, 512:1024], func=COPY), r=["psY1"], w=["yb%db" % sl])
        dma("sp", ys_d[b * 128:(b + 1) * 128, :], yb[sl][:], ["yb%da" % sl, "yb%db" % sl], [], "yst%d" % sl)
    S.barrier()

    y1 = [sbb("y1_%d" % i, [128, D]) for i in range(2)]
    y2 = [sbb("y2_%d" % i, [128, D]) for i in range(2)]
    x1l = [sbb("x1l%d" % i, [128, D]) for i in range(2)]
    mm = sbb("mm", [128, D])
    oo = [sbb("oo%d" % i, [128, D]) for i in range(2)]
    junk2 = sbb("junk2", [128, D], BF16)
    st2 = sbb("st2", [128, 4])
    for t in range(NT):
        sl = t % 2
        sq = (t * 128) // seqlen
        for k, yy in ((0, y1), (1, y2)):
            S.add("pool", lambda h, t=t, k=k, yy=yy, sl=sl: h.indirect_dma_start(
                out=yy[sl][:], out_offset=None, in_=ys_d[:, :],
                in_offset=bass.IndirectOffsetOnAxis(ap=dest_i[:, t, k:k + 1], axis=0)),
                r=["dest_i"], w=["y%d_%d" % (k, sl)], dma="yg%d_%d" % (k, sl))
        dma("sp", x1l[sl][:], x1s_d[t * 128:(t + 1) * 128, :], [], ["x1l%d" % sl], "x1ld%d" % sl)
        S.add("dve", lambda h, t=t, sl=sl: h.tensor_scalar(out=mm[:], in0=y1[sl][:], scalar1=gates[:, t, 0:1], scalar2=None, op0=ALU.mult),
              r=["y0_%d" % sl], w=["mm"])
        S.add("dve", lambda h, t=t, sl=sl: h.scalar_tensor_tensor(out=mm[:], in0=y2[sl][:], scalar=gates[:, t, 1:2], in1=mm[:],
                                                                  op0=ALU.mult, op1=ALU.add), r=["y1_%d" % sl, "mm"], w=["mm"])
        S.add("pool", lambda h, sq=sq: h.tensor_tensor(out=mm[:], in0=mm[:], in1=g2rep[:, sq, :], op=ALU.mult), r=["mm", "g2rep"], w=["mm"])
        S.add("pool", lambda h, sl=sl: h.tensor_tensor(out=mm[:], in0=mm[:], in1=x1l[sl][:], op=ALU.add), r=["mm", "x1l%d" % sl], w=["mm"])
        S.add("act", lambda h: h.activation(out=junk2[:], in_=mm[:], func=SQ, accum_out=st2[:, 0:1]), r=["mm", "junk2"], w=["junk2", "st2a"])
        rstd_ops(st2[:, 0:1], st2[:, 1:2], D, "st2a", "st2b")
        S.add("dve", lambda h, sl=sl: h.scalar_tensor_tensor(out=oo[sl][:], in0=mm[:], scalar=st2[:, 1:2], in1=fwrep[:],
                                                             op0=ALU.mult, op1=ALU.mult), r=["mm", "st2b"], w=["oo%d" % sl])
        dma("sp", out_d[t * 128:(t + 1) * 128, :], oo[sl][:], ["oo%d" % sl], [], "ost%d" % sl)
    return finish()


_CACHE = {}


def _prep_common(inp):
    f = np.float32
    w_in = np.asarray(inp["w_in"], f)[0]
    perm = np.concatenate([np.arange(0, 256), np.arange(1024, 2816), np.arange(256, 1024), np.arange(2816, 2828)])
    cw = np.asarray(inp["conv_w"], f)[0]
    grp = np.array([[2 * b + (1 if p >= 64 else 0) for b in range(2)] for p in range(128)])
    wins = np.array([2, 4, 8, 16])[grp]
    pcoef = np.zeros((128, 2, 4), f)
    for wi, w in enumerate((2, 4, 8, 16)):
        pcoef[:, :, wi] = np.where(wins == w, 1.0 / w, 0.0)
    pos = np.arange(1, 17, dtype=f)[None, None, :]
    pratio = (wins[:, :, None] / np.minimum(pos, wins[:, :, None])).astype(f)
    rep = lambda v: np.ascontiguousarray(np.broadcast_to(np.asarray(v, f).reshape(1, -1), (128, np.asarray(v).size)))
    return {
        "w_ada": np.ascontiguousarray(np.asarray(inp["w_ada"], f)[0]),
        "b_adaT": np.ascontiguousarray(np.asarray(inp["b_ada"], f)[0].reshape(48, 128).T),
        "w_in": np.ascontiguousarray(w_in[:, perm]),
        "w_pool": np.ascontiguousarray(np.asarray(inp["w_pool"], f)[0]),
        "pscale": np.ascontiguousarray(np.asarray(inp["pool_scale"], f)[0].reshape(2, 128).T),
        "convw": np.ascontiguousarray(cw.T.reshape(14, 128, 4).transpose(1, 0, 2).reshape(128, 56)),
        "convb": np.ascontiguousarray(np.asarray(inp["conv_b"], f)[0].reshape(14, 128).T),
        "dtb": rep(inp["dt_bias"]), "alog": rep(inp["a_log"]), "dskip": rep(inp["d_skip"]),
        "normw": np.ascontiguousarray(np.asarray(inp["ssd_norm_w"], f)[0].reshape(6, 128).T),
        "w_out": np.ascontiguousarray(np.asarray(inp["w_out"], f)[0]),
        "wgr": np.ascontiguousarray(np.concatenate([np.asarray(inp["w_group"], f)[0], np.asarray(inp["w_router"], f)[0]], axis=1)
                                    .reshape(8, 128, 36).transpose(1, 0, 2)),
        "bgr": rep(np.concatenate([np.asarray(inp["b_group"], f)[0], np.asarray(inp["b_router"], f)[0]])),
        "w13": np.ascontiguousarray(np.asarray(inp["w13"], f)[0]),
        "w2": np.ascontiguousarray(np.asarray(inp["w2"], f)[0]),
        "fw": rep(inp["final_norm_w"]),
        "pcoef": pcoef, "pratio": pratio,
    }


def _core_map(common, x, c, nseq, seqlen, i):
    f = np.float32
    xc_ = np.ascontiguousarray(np.asarray(x[i * nseq:(i + 1) * nseq], f).reshape(nseq * seqlen, D))
    cc = np.asarray(c[i * nseq:(i + 1) * nseq], f)
    cT = np.ascontiguousarray(cc.T.reshape(8, 128, nseq).transpose(1, 0, 2))
    m = dict(common)
    m["x"] = xc_
    m["cT"] = cT
    return m


def kernel(**inputs):
    x = np.asarray(inputs["x"])
    c = np.asarray(inputs["c"])
    B, SEQ, _ = x.shape
    nseq = B // NCORES
    key = (nseq, SEQ)
    if key not in _CACHE:
        _CACHE[key] = build(nseq, SEQ)[0]
    nc = _CACHE[key]
    common = _prep_common(inputs)
    in_maps = [_core_map(common, x, c, nseq, SEQ, i) for i in range(NCORES)]
    res = run_bass_kernel_spmd(nc, in_maps, core_ids=list(range(NCORES)))
    outs = [np.asarray(r["out"]).reshape(nseq, SEQ, D) for r in res.results]
    return np.concatenate(outs, axis=0).astype(np.float32)
```

```python
import numpy as np
import ml_dtypes
from contextlib import ExitStack
import concourse.bass as bass
import concourse.mybir as mybir
from concourse.bass_utils import run_bass_kernel_spmd

F32 = mybir.dt.float32
BF16 = mybir.dt.bfloat16
I32 = mybir.dt.int32
ALU = mybir.AluOpType
AF = mybir.ActivationFunctionType
AX = mybir.AxisListType

D = 1024
KC = 8
INP = 2828
NE = 32
FF = 512
NCORES = 8
EPS = 1e-6
BIG = 30000.0
C_U, C_XBC, C_Z, C_DT = 0, 256, 2048, 2816


class _Op:
    __slots__ = ("eng", "fn", "deps", "dma", "dma_idx", "sig", "sigval", "idx", "waits", "dma_need")


class _Rec:
    def __getattr__(self, name):
        def f(*a, **k):
            self.__dict__["call"] = (name, a, k)
            return self
        return f


class Sched:
    ENGS = ("pe", "act", "dve", "pool", "sp")

    def __init__(self):
        self.ops = {e: [] for e in self.ENGS}
        self.last_w = {}
        self.readers = {}
        self.dma_count = {}
        self.dma_last = {}
        self.pending = {e: [] for e in self.ENGS}

    def add(self, eng, fn, r=(), w=(), dma=None):
        o = _Op()
        rec = _Rec()
        fn(rec)
        o.eng, o.fn, o.dma, o.sig, o.sigval, o.dma_idx = eng, rec.call, dma, False, 0, 0
        deps = [(d, True) for d in self.pending[eng]]
        self.pending[eng] = []
        for res in r:
            lw = self.last_w.get(res)
            if lw is not None:
                deps.append((lw, True))
        for res in w:
            rd = self.readers.get(res, ())
            lw = self.last_w.get(res)
            if lw is not None and not rd:
                deps.append((lw, False))
            deps.extend((x, False) for x in rd)
        o.deps = [(d, raw) for d, raw in deps if d.dma is None]
        o.dma_need = {d.dma: self.dma_count[d.dma] for d, raw in deps if d.dma is not None}
        if dma is not None:
            self.dma_count[dma] = self.dma_count.get(dma, 0) + 1
            o.dma_idx = self.dma_count[dma]
            self.dma_last[dma] = o
        for res in r:
            self.readers.setdefault(res, []).append(o)
        for res in w:
            self.last_w[res] = o
            self.readers[res] = []
        o.idx = len(self.ops[eng])
        self.ops[eng].append(o)
        return o

    def barrier(self):
        deps = []
        for e in self.ENGS:
            if self.ops[e]:
                deps.append(self.ops[e][-1])
        deps.extend(self.dma_last.values())
        for e in self.ENGS:
            self.pending[e] = list(deps)

    def finalize(self):
        for e in self.ENGS:
            seen_eng = {}
            seen_dma = {}
            for o in self.ops[e]:
                need_eng = {}
                need_dma = dict(o.dma_need)
                for d, raw in o.deps:
                    if d is o:
                        continue
                    if d.dma is not None:
                        if d.dma_idx > need_dma.get(d.dma, 0):
                            need_dma[d.dma] = d.dma_idx
                    else:
                        if d.eng == e and e == "pe":
                            continue
                        if d.eng == e and d.idx >= o.idx:
                            continue
                        cur = need_eng.get(d.eng)
                        if cur is None or d.idx > cur.idx:
                            need_eng[d.eng] = d
                waits = []
                for k, v in need_dma.items():
                    if v > seen_dma.get(k, 0):
                        seen_dma[k] = v
                        waits.append(("dma", k, v))
                for k, d in need_eng.items():
                    if d.idx > seen_eng.get(k, -1):
                        seen_eng[k] = d.idx
                        d.sig = True
                        waits.append(("eng", k, d))
                o.waits = waits
        self.n_epochs = {}
        for e in self.ENGS:
            c = 0
            for o in self.ops[e]:
                if o.sig and o.dma is None:
                    c += 1
                    o.sigval = c
            self.n_epochs[e] = (c + self.EP - 1) // self.EP if c else 0

    EP = 1000
    DEP = 64

    def emit(self, nc, stack):
        self.finalize()
        esem = {e: [stack.enter_context(nc.semaphore("s_%s%d" % (e, i))) for i in range(self.n_epochs[e])]
                for e in self.ENGS}
        dsem = {k: [stack.enter_context(nc.semaphore("d_%s_%d" % (k, i))) for i in range((n + self.DEP - 1) // self.DEP)]
                for k, n in self.dma_count.items()}
        block = stack.enter_context(nc.Block())
        EP, DEP = self.EP, self.DEP

        def run(e, h):
            for o in self.ops[e]:
                for kind, k, v in o.waits:
                    if kind == "dma":
                        h.wait_ge(dsem[k][(v - 1) // DEP], 16 * ((v - 1) % DEP + 1))
                    else:
                        h.wait_ge(esem[k][(v.sigval - 1) // EP], (v.sigval - 1) % EP + 1)
                name, a, k = o.fn
                try:
                    ins = getattr(h, name)(*a, **k)
                except Exception:
                    print("EMIT FAIL", e, name, {kk: str(vv)[:300] for kk, vv in k.items()}, flush=True)
                    raise
                if o.dma is not None:
                    ins.then_inc(dsem[o.dma][(o.dma_idx - 1) // DEP], 16)
                elif o.sig:
                    ins.then_inc(esem[e][(o.sigval - 1) // EP], 1)

        @block.tensor
        def _(h):
            run("pe", h)

        @block.scalar
        def _(h):
            run("act", h)

        @block.vector
        def _(h):
            run("dve", h)

        @block.gpsimd
        def _(h):
            run("pool", h)

        @block.sync
        def _(h):
            run("sp", h)


def build(nseq=4, seqlen=2048, debug=()):
    T = nseq * seqlen
    NT = T // 128
    TM = 256
    CPM = TM // 128
    NMT = seqlen // TM
    R = 2 * T + NE * 128
    NB = R // 128
    nc = bass.Bass("TRN2", target_bir_lowering=False)
    S = Sched()
    st = ExitStack()

    def din(name, shape, dt=F32):
        return nc.dram_tensor(name, list(shape), dt, kind="ExternalInput").ap()

    x_d = din("x", [T, D])
    cT_d = din("cT", [128, KC, nseq])
    wada_d = din("w_ada", [D, 6 * D])
    badaT_d = din("b_adaT", [128, 48])
    win_d = din("w_in", [D, INP])
    wpool_d = din("w_pool", [4, 64, 64])
    pscale_d = din("pscale", [128, 2])
    convw_d = din("convw", [128, 56])
    convb_d = din("convb", [128, 14])
    dtb_d = din("dtb", [128, 12])
    alog_d = din("alog", [128, 12])
    dskip_d = din("dskip", [128, 12])
    normw_d = din("normw", [128, 6])
    wout_d = din("w_out", [D, D])
    wgr_d = din("wgr", [128, KC, 36])
    bgr_d = din("bgr", [128, 36])
    w13_d = din("w13", [NE, D, 2 * FF])
    w2_d = din("w2", [NE, FF, D])
    fw_d = din("fw", [128, D])
    pcoef_d = din("pcoef", [128, 2, 4])
    pratio_d = din("pratio", [128, 2, 16])
    out_d = nc.dram_tensor("out", [T, D], F32, kind="ExternalOutput").ap()
    x1s_d = nc.dram_tensor("x1s", [T, D], F32).ap()
    h2s_d = nc.dram_tensor("h2s", [T, D], BF16).ap()
    xs_d = nc.dram_tensor("xs", [R, D], BF16).ap()
    ys_d = nc.dram_tensor("ys", [R, D], F32).ap()
    dbg_out = {}

    def sb(name, shape, dt=F32, stack=st):
        return stack.enter_context(nc.sbuf_tensor("sb_" + name, list(shape), dt))

    def ps(name, shape, dt=F32):
        return st.enter_context(nc.psum_tensor("ps_" + name, list(shape), dt))

    def dma(eng, out, in_, r, w, key):
        return S.add(eng, lambda h, o=out, i=in_: h.dma_start(out=o, in_=i), r=r, w=w, dma=key)

    def dump(name, ap, shape, res, dt=F32):
        d = nc.dram_tensor("dbg_" + name, list(shape), dt, kind="ExternalOutput").ap()
        dbg_out[name] = d
        dma("sp", d, ap, res, [], "dbg")

    def finish():
        S.barrier()
        S.add("sp", lambda h: h.nop(), r=[], w=[])
        S.emit(nc, st)
        return nc, dbg_out

    psT = ps("psT", [128, 512])
    psW = ps("psW", [128, 1024])
    psSG = ps("psSG", [128, 1024])
    psY = ps("psY", [128, 1024])
    psS = ps("psS", [128, 512])
    psT_bf = psT[:, :].bitcast(BF16)

    ident_f = sb("ident_f", [128, 128])
    ident_b = sb("ident_b", [128, 128], BF16)
    ones_f = sb("ones_f", [128, 128])
    ones_b = sb("ones_b", [128, 128], BF16)
    mJL = sb("mJL", [128, 128])
    mST = sb("mST", [128, 128])
    mLT_b = sb("mLT_b", [128, 128], BF16)
    iota_p = sb("iota_p", [128, 1])
    iota_e = sb("iota_e", [128, 32])
    modp = sb("modp", [128, 48, nseq])
    gates = sb("gates", [128, NT, 2])
    eidx = sb("eidx", [128, NT, 2])
    posk = sb("posk", [128, NT, 2])
    totrun = sb("totrun", [128, 32])
    dest_i = sb("dest_i", [128, NT, 2], I32)
    widx13_i = sb("widx13_i", [128, NB, 4], I32)
    widx2_i = sb("widx2_i", [128, NB, 2], I32)
    fwrep = sb("fwrep", [128, D])

    stA = ExitStack()

    def sba(name, shape, dt=F32):
        return sb(name, shape, dt, stack=stA)

    win_b = sba("win_b", [128, KC, INP], BF16)
    wout_b = sba("wout_b", [128, KC, D], BF16)
    cdiag = sba("cdiag", [128, 56, 128], BF16)
    ddiag = sba("ddiag", [128, 12, 128], BF16)
    wpool_b = sba("wpool_b", [128, 2, 128], BF16)
    wgr = sba("wgr", [128, KC, 36])
    bgr = sba("bgr", [128, 36])
    pscale = sba("pscale", [128, 2])
    convw = sba("convw", [128, 56])
    convb = sba("convb", [128, 14])
    dtb = sba("dtb", [128, 12])
    arep = sba("arep", [128, 12])
    dskip = sba("dskip", [128, 12])
    normw = sba("normw", [128, 6])
    pcoef = sba("pcoef", [128, 2, 4])
    pratio = sba("pratio", [128, 2, 16])
    cT = sba("cT", [128, KC, nseq])
    badaT = sba("badaT", [128, 48])
    stI = ExitStack()
    stage = sb("stage", [128, 6144], stack=stI)
    stage2 = sb("stage2", [128, 6144], stack=stI)

    for t_, d_ in ((wgr, wgr_d), (bgr, bgr_d), (pscale, pscale_d), (convb, convb_d), (dtb, dtb_d), (arep, alog_d),
                   (dskip, dskip_d), (normw, normw_d), (pcoef, pcoef_d), (pratio, pratio_d), (convw, convw_d),
                   (cT, cT_d), (badaT, badaT_d), (fwrep, fw_d)):
        dma("sp", t_[:], d_, [], [], "init")

    S.add("pool", lambda h: h.memset(ones_f[:], 1.0), w=["ones_f"])
    S.add("pool", lambda h: h.memset(ones_b[:], 1.0), w=["ones_b"])
    S.add("pool", lambda h: h.affine_select(out=ident_f[:], in_=ones_f[:], pattern=[[-1, 128]],
                                            compare_op=ALU.is_equal, fill=0.0, base=0, channel_multiplier=1),
          r=["ones_f"], w=["ident_f"])
    S.add("pool", lambda h: h.affine_select(out=mJL[:], in_=ones_f[:], pattern=[[1, 128]],
                                            compare_op=ALU.is_ge, fill=0.0, base=0, channel_multiplier=-1),
          r=["ones_f"], w=["mJL"])
    S.add("pool", lambda h: h.affine_select(out=mST[:], in_=ones_f[:], pattern=[[-1, 128]],
                                            compare_op=ALU.is_gt, fill=0.0, base=0, channel_multiplier=1),
          r=["ones_f"], w=["mST"])
    S.add("pool", lambda h: h.affine_select(out=mLT_b[:], in_=ones_b[:], pattern=[[1, 128]],
                                            compare_op=ALU.is_gt, fill=0.0, base=0, channel_multiplier=-1),
          r=["ones_b"], w=["mLT_b"])
    S.add("pool", lambda h: h.tensor_copy(out=ident_b[:], in_=ident_f[:]), r=["ident_f"], w=["ident_b"])
    S.add("pool", lambda h: h.iota(iota_p[:], pattern=[[0, 1]], base=0, channel_multiplier=1,
                                   allow_small_or_imprecise_dtypes=True), w=["iota_p"])
    S.add("pool", lambda h: h.iota(iota_e[:], pattern=[[1, 32]], base=0, channel_multiplier=0,
                                   allow_small_or_imprecise_dtypes=True), w=["iota_e"])
    S.add("pool", lambda h: h.memset(totrun[:], 0.0), w=["totrun"])
    S.add("pool", lambda h: h.memset(stage2[:, 0:256], 0.0), w=["stage2"])
    S.barrier()

    S.add("act", lambda h: h.activation(out=arep[:], in_=arep[:], func=AF.Exp), r=[], w=["arep"])
    S.add("dve", lambda h: h.tensor_scalar(out=arep[:], in0=arep[:], scalar1=-1.0, scalar2=None, op0=ALU.mult),
          r=["arep"], w=["arep"])
    S.add("dve", lambda h: h.tensor_tensor(out=cdiag[:], in0=ident_f[:, None, :].to_broadcast([128, 56, 128]),
                                           in1=convw[:, :].unsqueeze(2).to_broadcast([128, 56, 128]), op=ALU.mult),
          r=[], w=["cdiag"])
    S.add("dve", lambda h: h.tensor_tensor(out=ddiag[:], in0=ident_f[:, None, :].to_broadcast([128, 12, 128]),
                                           in1=dskip[:, :].unsqueeze(2).to_broadcast([128, 12, 128]), op=ALU.mult),
          r=[], w=["ddiag"])
    for g in range(4):
        blk, half = g // 2, g % 2
        dma("sp", stage2[half * 64:(half + 1) * 64, blk * 128 + half * 64: blk * 128 + half * 64 + 64],
            wpool_d[g], [], [], "init2")
    S.barrier()
    S.add("dve", lambda h: h.tensor_copy(out=wpool_b[:], in_=stage2[:, 0:256].rearrange("p (b c) -> p b c", b=2)),
          r=[], w=["wpool_b", "stage2"])
    S.add("act", lambda h: h.activation(out=cT[:], in_=cT[:], func=AF.Silu), r=[], w=["cT"])
    stg = [stage, stage2]
    stn = ["stage", "stage2"]
    psM = psW[:, 0:48 * nseq]
    wada_v = wada_d.rearrange("(kc p) n -> p kc n", p=128)
    for jg in range(12):
        si = jg % 2
        sg, rs = stg[si], stn[si]
        sgv = sg[:, 0:4096].rearrange("p (kc n) -> p kc n", n=512)
        dma("sp", sgv, wada_v[:, :, jg * 512:(jg + 1) * 512], [], [rs], "wst%d" % si)
        for jl in range(4):
            j = jg * 4 + jl
            for kc in range(KC):
                S.add("pe", lambda h, sgv=sgv, j=j, jl=jl, kc=kc: h.matmul(
                    psW[:, j * nseq:(j + 1) * nseq], lhsT=sgv[:, kc, jl * 128:(jl + 1) * 128], rhs=cT[:, kc, :],
                    start=(kc == 0), stop=(kc == KC - 1)), r=[rs, "cT"], w=["psW0"])
    S.add("dve", lambda h: h.tensor_tensor(out=modp[:], in0=psM.rearrange("p (j s) -> p j s", s=nseq),
                                           in1=badaT[:, :].unsqueeze(2).to_broadcast([128, 48, nseq]), op=ALU.add),
          r=["psW0"], w=["modp"])
    for j0 in (8, 32):
        S.add("dve", lambda h, j0=j0: h.tensor_scalar(out=modp[:, j0:j0 + 8, :], in0=modp[:, j0:j0 + 8, :],
                                                       scalar1=1.0, scalar2=None, op0=ALU.add),
              r=["modp"], w=["modp"])
    cast_engs = ["dve", "pool", "act"]
    ci = 0
    for kc in range(KC):
        si = kc % 2
        sg, rs = stg[si], stn[si]
        dma("sp", sg[:, 0:INP], win_d[kc * 128:(kc + 1) * 128, :], [], [rs], "wst%d" % si)
        for (a, b) in ((0, 1024), (1024, 2048), (2048, INP)):
            eng = cast_engs[ci % 3]
            ci += 1
            if eng == "act":
                S.add("act", lambda h, sg=sg, kc=kc, a=a, b=b: h.activation(out=win_b[:, kc, a:b], in_=sg[:, a:b],
                                                                            func=AF.Copy), r=[rs], w=[])
            else:
                S.add(eng, lambda h, sg=sg, kc=kc, a=a, b=b: h.tensor_copy(out=win_b[:, kc, a:b], in_=sg[:, a:b]),
                      r=[rs], w=[])
    for kc in range(KC):
        si = kc % 2
        sg, rs = stg[si], stn[si]
        dma("sp", sg[:, 0:D], wout_d[kc * 128:(kc + 1) * 128, :], [], [rs], "wst%d" % si)
        eng = cast_engs[kc % 3]
        if eng == "act":
            S.add("act", lambda h, sg=sg, kc=kc: h.activation(out=wout_b[:, kc, :], in_=sg[:, 0:D], func=AF.Copy),
                  r=[rs], w=[])
        else:
            S.add(eng, lambda h, sg=sg, kc=kc: h.tensor_copy(out=wout_b[:, kc, :], in_=sg[:, 0:D]), r=[rs], w=[])
    S.barrier()
    if "I" in debug:
        dump("modp", modp[:], [128, 48, nseq], [])
        dump("ident", ident_f[:], [128, 128], [])
        dump("mJL", mJL[:], [128, 128], [])
        return finish()
    stI.close()

    repA = sba("repA", [128, 3, D])
    dg = sba("dg", [128, 4, 128])
    xt = [sba("xt%d" % i, [128, D]) for i in range(2)]
    junk = sba("junk", [128, D], BF16)
    xn = sba("xn", [128, D], BF16)
    hT = sba("hT", [128, KC, TM], BF16)
    ubuf = sba("ubuf", [128, 2, 16 + TM])
    sA = sba("sA", [128, 2, 16 + TM])
    sB = sba("sB", [128, 2, 16 + TM])
    pmean = sba("pmean", [128, 2, TM])
    pdiff = sba("pdiff", [128, 2, TM], BF16)
    xpre = sba("xpre", [128, 14, 4 + TM], BF16)
    xact = sba("xact", [128, 14, TM], BF16)
    mixT = sba("mixT", [128, KC, TM], BF16)
    ctmp = sba("ctmp", [128, 2, 16])
    ctmp2 = sba("ctmp2", [128, 14, 4])
    sz = sba("sz", [128, 768])
    sm = sba("sm", [128, 8, 12])
    cum = sba("cum", [128, 24])
    e24 = sba("e24", [128, 24])
    rhsda = [sba("rhsda%d" % i, [128, 3, 128]) for i in range(2)]
    xc = sba("xc", [128, 768], BF16)
    xdte = sba("xdte", [128, 768], BF16)
    xtm = sba("xtm", [128, 768], BF16)
    btm = sba("btm", [128, 512], BF16)
    cbm = [sba("cbm%d" % i, [128, 128]) for i in range(2)]
    mt = [sba("mt%d" % i, [128, 3, 128], BF16) for i in range(2)]
    ytmp = [sba("ytmp%d" % i, [128, 192]) for i in range(2)]
    ych = sba("ych", [128, 768])
    ygb = sba("ygb", [128, 768], BF16)
    Sst = sba("Sst", [128, 4, 192])
    Sbf = sba("Sbf", [128, 4, 192], BF16)
    x1 = sba("x1", [128, D])
    xn2 = sba("xn2", [128, D])
    h2t = sba("h2t", [128, D])
    h2b = sba("h2b", [128, D], BF16)
    h2T = sba("h2T", [128, KC, 128])
    stat = sba("stat", [128, 16])
    rt = sba("rt", [128, 8, 36])
    abf = sba("abf", [128, 32], BF16)

    SQ, EXP, LN, SILU, COPY = AF.Square, AF.Exp, AF.Ln, AF.Silu, AF.Copy

    def rstd_ops(ss_ap, out_ap, n, res_in, res_out):
        S.add("dve", lambda h: h.tensor_scalar(out=out_ap, in0=ss_ap, scalar1=1.0 / n, scalar2=EPS,
                                               op0=ALU.mult, op1=ALU.add), r=[res_in], w=[res_out])
        S.add("act", lambda h: h.activation(out=out_ap, in_=out_ap, func=LN), r=[res_out], w=[res_out])
        S.add("act", lambda h: h.activation(out=out_ap, in_=out_ap, func=EXP, scale=-0.5), r=[res_out], w=[res_out])

    def rep_rows(dst_fn, j0, sidx, res_w):
        for q in range(2):
            S.add("dve", lambda h, q=q: h.tensor_tensor(
                out=dg[:], in0=ident_f[:, None, :].to_broadcast([128, 4, 128]),
                in1=modp[:, j0 + 4 * q:j0 + 4 * q + 4, sidx:sidx + 1].to_broadcast([128, 4, 128]), op=ALU.mult),
                r=["modp"], w=["dg"])
            S.add("pe", lambda h: h.matmul(psW[:, 0:512], lhsT=ones_f[:], rhs=dg[:].rearrange("p a b -> p (a b)"),
                                           start=True, stop=True), r=["dg"], w=["psW0"])
            S.add("act", lambda h, q=q: h.activation(out=dst_fn(q), in_=psW[:, 0:512], func=COPY),
                  r=["psW0"], w=[res_w])

    XPRE_ALL = ["xpre%d" % b for b in range(14)]
    tile_i = 0
    for s in range(nseq):
        for gi, j0 in enumerate((16, 24, 32)):
            rep_rows(lambda q, gi=gi: repA[:, gi, q * 512:(q + 1) * 512], j0, s, "repA")
        S.add("dve", lambda h: h.memset(ubuf[:, :, 0:16], 0.0), w=["ubuf"])
        S.add("dve", lambda h: h.memset(xpre[:, :, 0:4], 0.0), w=XPRE_ALL)
        S.add("pool", lambda h: h.memset(Sst[:], 0.0), w=["Sst%d" % g_ for g_ in range(4)])
        S.add("dve", lambda h: h.memset(Sbf[:], 0.0), w=["Sbf%d" % g_ for g_ in range(4)])

        for m in range(NMT):
            tok0 = s * seqlen + m * TM
            if "A0" in debug:
                S.barrier()
                dump("repA", repA[:], [128, 3, D], [])
                return finish()
            for c in range(CPM):
                ti = tile_i + c
                xs_ = xt[ti % 2]
                xr = "xt%d" % (ti % 2)
                dma("sp", xs_[:], x_d[tok0 + c * 128: tok0 + (c + 1) * 128, :], [], [xr], "xl%d" % (ti % 2))
                S.add("act", lambda h, xs_=xs_: h.activation(out=junk[:], in_=xs_[:], func=SQ, accum_out=stat[:, 0:1]),
                      r=[xr, "junk"], w=["junk", "stat0"])
                rstd_ops(stat[:, 0:1], stat[:, 1:2], D, "stat0", "stat1")
                S.add("act", lambda h, xs_=xs_: h.activation(out=xn[:], in_=xs_[:], func=COPY, scale=stat[:, 1:2]),
                      r=[xr, "stat1"], w=["xn"])
                for kc in range(KC):
                    S.add("pe", lambda h, kc=kc: h.transpose(psT_bf[:, kc * 128:(kc + 1) * 128],
                                                             xn[:, kc * 128:(kc + 1) * 128], ident_b[:]),
                          r=["xn"], w=["psT"])
                for kc in range(KC):
                    S.add("dve", lambda h, kc=kc, c=c: h.tensor_scalar(
                        out=hT[:, kc, c * 128:(c + 1) * 128], in0=psT_bf[:, kc * 128:(kc + 1) * 128],
                        scalar1=modp[:, 8 + kc, s:s + 1], scalar2=modp[:, kc, s:s + 1], op0=ALU.mult, op1=ALU.add),
                        r=["psT"], w=["hT"])
            if "A1" in debug:
                S.barrier()
                dump("hT", hT[:], [128, KC, TM], [], BF16)
                return finish()
            for blk in range(16):
                col0 = C_U + blk * 128
                bank = blk % 2
                pw = psW[:, bank * 512:bank * 512 + TM]
                for kc in range(KC):
                    S.add("pe", lambda h, kc=kc, col0=col0, pw=pw: h.matmul(
                        pw, lhsT=win_b[:, kc, col0:col0 + 128], rhs=hT[:, kc, :], start=(kc == 0), stop=(kc == KC - 1)),
                        r=["hT"], w=["psW%d" % bank])
                if blk < 2:
                    S.add("act", lambda h, blk=blk, pw=pw: h.activation(out=ubuf[:, blk, 16:16 + TM], in_=pw, func=COPY),
                          r=["psW%d" % bank], w=["ubuf"])
                elif blk % 2:
                    S.add("dve", lambda h, blk=blk, pw=pw: h.tensor_copy(out=xpre[:, blk - 2, 4:4 + TM], in_=pw),
                          r=["psW%d" % bank], w=["xpre%d" % (blk - 2)])
                else:
                    S.add("act", lambda h, blk=blk, pw=pw: h.activation(out=xpre[:, blk - 2, 4:4 + TM], in_=pw, func=COPY),
                          r=["psW%d" % bank], w=["xpre%d" % (blk - 2)])
            if "A15" in debug:
                S.barrier()
                dump("ubuf", ubuf[:, :, 16:16 + TM], [128, 2, TM], [])
                dump("xpre", xpre[:, :, 4:4 + TM], [128, 14, TM], [], BF16)
                return finish()
            E = 16 + TM
            S.add("pool", lambda h: h.tensor_tensor(out=sA[:, :, 1:E], in0=ubuf[:, :, 1:E], in1=ubuf[:, :, 0:E - 1], op=ALU.add),
                  r=["ubuf"], w=["sA"])
            for blk in range(2):
                S.add("dve", lambda h, blk=blk: h.tensor_scalar(out=pmean[:, blk, :], in0=sA[:, blk, 16:E],
                                                                 scalar1=pcoef[:, blk, 0:1], scalar2=None, op0=ALU.mult),
                      r=["sA"], w=["pmean"])
            prev, prevn = sA, "sA"
            for wi, sh in ((1, 2), (2, 4), (3, 8)):
                cur, curn = (sB, "sB") if prev is sA else (sA, "sA")
                lo = 2 * sh - 1
                S.add("pool", lambda h, cur=cur, prev=prev, lo=lo, sh=sh: h.tensor_tensor(
                    out=cur[:, :, lo:E], in0=prev[:, :, lo:E], in1=prev[:, :, lo - sh:E - sh], op=ALU.add),
                    r=[prevn], w=[curn])
                for blk in range(2):
                    S.add("dve", lambda h, cur=cur, wi=wi, blk=blk: h.scalar_tensor_tensor(
                        out=pmean[:, blk, :], in0=cur[:, blk, 16:E], scalar=pcoef[:, blk, wi:wi + 1],
                        in1=pmean[:, blk, :], op0=ALU.mult, op1=ALU.add), r=[curn, "pmean"], w=["pmean"])
                prev, prevn = cur, curn
            if m == 0:
                S.add("dve", lambda h: h.tensor_tensor(out=pmean[:, :, 0:16], in0=pmean[:, :, 0:16], in1=pratio[:], op=ALU.mult),
                      r=["pmean"], w=["pmean"])
            S.add("dve", lambda h: h.tensor_tensor(out=pdiff[:], in0=pmean[:], in1=ubuf[:, :, 16:E], op=ALU.subtract),
                  r=["pmean", "ubuf"], w=["pdiff"])
            S.add("dve", lambda h: h.tensor_copy(out=ctmp[:], in_=ubuf[:, :, TM:E]), r=["ubuf"], w=["ctmp"])
            S.add("dve", lambda h: h.tensor_copy(out=ubuf[:, :, 0:16], in_=ctmp[:]), r=["ctmp"], w=["ubuf"])
            def pool_mm():
                for blk in range(2):
                    pw = psW[:, blk * 512:blk * 512 + TM]
                    S.add("pe", lambda h, blk=blk, pw=pw: h.matmul(pw, lhsT=wpool_b[:, blk, :], rhs=pdiff[:, blk, :],
                                                                   start=True, stop=True), r=["pdiff"], w=["psW%d" % blk])
                    S.add("act", lambda h, blk=blk, pw=pw: h.activation(out=mixT[:, blk, :], in_=pw, func=COPY,
                                                                        scale=pscale[:, blk:blk + 1]),
                          r=["psW%d" % blk], w=["mixT_p"])

            if "A17" in debug:
                S.barrier()
                dump("mixTp", mixT[:, 0:2, :], [128, 2, TM], [], BF16)
                return finish()
            for blk in range(14):
                bank = blk % 2
                pw = psW[:, bank * 512:bank * 512 + TM]
                for k in range(4):
                    S.add("pe", lambda h, blk=blk, k=k, pw=pw: h.matmul(
                        pw, lhsT=cdiag[:, blk * 4 + k, :], rhs=xpre[:, blk, 1 + k:1 + k + TM], start=(k == 0), stop=(k == 3)),
                        r=["xpre%d" % blk], w=["psW%d" % bank])
                S.add("act", lambda h, blk=blk, pw=pw: h.activation(out=xact[:, blk, :], in_=pw, func=SILU,
                                                                    bias=convb[:, blk:blk + 1]),
                      r=["psW%d" % bank], w=["xact"])
            if "A18" in debug:
                S.barrier()
                dump("xact", xact[:], [128, 14, TM], [], BF16)
                return finish()
            S.add("dve", lambda h: h.tensor_copy(out=ctmp2[:], in_=xpre[:, :, TM:TM + 4]), r=XPRE_ALL, w=["ctmp2"])
            S.add("dve", lambda h: h.tensor_copy(out=xpre[:, :, 0:4], in_=ctmp2[:]), r=["ctmp2"], w=XPRE_ALL)

            if "A2" in debug:
                S.barrier()
                dump("hT", hT[:], [128, KC, TM], [], BF16)
                dump("xact", xact[:], [128, 14, TM], [], BF16)
                dump("mixTp", mixT[:, 0:2, :], [128, 2, TM], [], BF16)
                return finish()
            for c in range(CPM):
                ti = tile_i + c
                cs = slice(c * 128, (c + 1) * 128)
                xs_ = xt[ti % 2]
                xr = "xt%d" % (ti % 2)
                for (o0, o1, w0) in ((0, 512, C_Z), (512, 768, C_Z + 512)):
                    bank = o0 // 512
                    for kc in range(KC):
                        S.add("pe", lambda h, kc=kc, o0=o0, o1=o1, w0=w0: h.matmul(
                            psW[:, o0:o1], lhsT=hT[:, kc, cs], rhs=win_b[:, kc, w0:w0 + (o1 - o0)],
                            start=(kc == 0), stop=(kc == KC - 1)), r=["hT"], w=["psW%d" % bank])
                for kc in range(KC):
                    S.add("pe", lambda h, kc=kc: h.matmul(psS[:, 24:36], lhsT=hT[:, kc, cs], rhs=win_b[:, kc, C_DT:C_DT + 12],
                                                           start=(kc == 0), stop=(kc == KC - 1)), r=["hT"], w=["psS"])
                S.add("act", lambda h: h.activation(out=sz[:], in_=psW[:, 0:768], func=SILU), r=["psW0", "psW1"], w=["sz"])
                v, aa, ee, dtc, da, dte, dtdte = (sm[:, i, :] for i in range(7))
                S.add("dve", lambda h: h.tensor_tensor(out=v, in0=psS[:, 24:36], in1=dtb[:], op=ALU.add), r=["psS"], w=["sm0"])
                S.add("dve", lambda h: h.tensor_scalar(out=aa, in0=v, scalar1=0.0, scalar2=-2.0, op0=ALU.max, op1=ALU.mult), r=["sm0"], w=["sm1"])
                S.add("dve", lambda h: h.tensor_tensor(out=aa, in0=aa, in1=v, op=ALU.add), r=["sm0", "sm1"], w=["sm1"])
                S.add("act", lambda h: h.activation(out=ee, in_=aa, func=EXP), r=["sm1"], w=["sm2"])
                S.add("act", lambda h: h.activation(out=ee, in_=ee, func=LN, bias=1.0), r=["sm2"], w=["sm2"])
                S.add("dve", lambda h: h.scalar_tensor_tensor(out=dtc, in0=v, scalar=0.0, in1=ee, op0=ALU.max, op1=ALU.add),
                      r=["sm0", "sm2"], w=["sm3"])
                S.add("dve", lambda h: h.tensor_tensor(out=da, in0=dtc, in1=arep[:], op=ALU.mult), r=["sm3"], w=["sm4"])
                S.add("pe", lambda h: h.matmul(psS[:, 0:12], lhsT=mJL[:], rhs=da, start=True, stop=True), r=["sm4"], w=["psS"])
                S.add("pe", lambda h: h.matmul(psS[:, 12:24], lhsT=ones_f[:], rhs=da, start=True, stop=True), r=["sm4"], w=["psS"])
                S.add("dve", lambda h: h.tensor_copy(out=cum[:], in_=psS[:, 0:24]), r=["psS"], w=["cum"])
                S.add("act", lambda h: h.activation(out=e24[:], in_=cum[:], func=EXP), r=["cum"], w=["e24"])
                S.add("dve", lambda h: h.tensor_tensor(out=dte, in0=cum[:, 12:24], in1=cum[:, 0:12], op=ALU.subtract), r=["cum"], w=["sm5"])
                S.add("act", lambda h: h.activation(out=dte, in_=dte, func=EXP), r=["sm5"], w=["sm5"])
                S.add("dve", lambda h: h.tensor_tensor(out=dtdte, in0=dte, in1=dtc, op=ALU.mult), r=["sm5", "sm3"], w=["sm6"])
                if c == 0 and "C1" in debug:
                    S.barrier()
                    dump('sm', sm[:], [128, 8, 12], [])
                    dump('e24', e24[:], [128, 24], [])
                    dump('sz', sz[:], [128, 768], [])
                    return finish()
                for b6 in range(6):
                    S.add("pe", lambda h, b6=b6: h.transpose(psT_bf[:, b6 * 128:(b6 + 1) * 128], xact[:, b6, cs], ident_b[:]),
                          r=["xact"], w=["psT"])
                psB = psW[:, 768:1024].bitcast(BF16)
                for g in range(4):
                    S.add("pe", lambda h, g=g: h.transpose(psB[:, g * 128:(g + 1) * 128], xact[:, 6 + g, cs], ident_b[:]),
                          r=["xact"], w=["psW1"])
                pxv = psT_bf[:, 0:768].rearrange("p (a b) -> p a b", b=64)
                S.add("dve", lambda h: h.tensor_tensor(out=xc[:].rearrange("p (a b) -> p a b", b=64), in0=pxv,
                                                       in1=dtc.unsqueeze(2).to_broadcast([128, 12, 64]), op=ALU.mult),
                      r=["psT", "sm3"], w=["xc"])
                S.add("dve", lambda h: h.tensor_tensor(out=xdte[:].rearrange("p (a b) -> p a b", b=64), in0=pxv,
                                                       in1=dtdte.unsqueeze(2).to_broadcast([128, 12, 64]), op=ALU.mult),
                      r=["psT", "sm6"], w=["xdte"])
                S.add("dve", lambda h: h.tensor_scalar(out=xtm[:], in0=psT_bf[:, 0:768], scalar1=1.0, scalar2=None, op0=ALU.mult),
                      r=["psT"], w=["xtm"])
                S.add("dve", lambda h: h.tensor_scalar(out=btm[:], in0=psB, scalar1=1.0, scalar2=None, op0=ALU.mult),
                      r=["psW1"], w=["btm"])
                if c == 0 and "C2" in debug:
                    S.barrier()
                    dump('xc', xc[:], [128, 768], [], BF16)
                    dump('btm', btm[:], [128, 512], [], BF16)
                    return finish()
                def stage_p(g):
                    gb = g % 2
                    sg_ = psSG[:, gb * 512:(gb + 1) * 512]
                    sgr = "psSG%d" % gb
                    S.add("pool", lambda h: h.affine_select(
                        out=rhsda[gb][:], in_=sm[:, 4, 3 * g:3 * g + 3].unsqueeze(2).to_broadcast([128, 3, 128]),
                        pattern=[[0, 3], [1, 128]], compare_op=ALU.is_ge, fill=0.0, base=0, channel_multiplier=-1),
                        r=["sm4"], w=["rhsda%d" % gb])
                    S.add("pe", lambda h: h.matmul(sg_[:, 0:384], lhsT=mST[:], rhs=rhsda[gb][:].rearrange("p a b -> p (a b)"),
                                                   start=True, stop=True), r=["rhsda%d" % gb], w=[sgr])
                    S.add("pe", lambda h: h.matmul(sg_[:, 384:512], lhsT=xact[:, 6 + g, cs], rhs=xact[:, 10 + g, cs],
                                                   start=True, stop=True), r=["xact"], w=[sgr])
                    S.add("act", lambda h: h.activation(out=sg_[:, 0:384], in_=sg_[:, 0:384], func=EXP), r=[sgr], w=[sgr])
                    S.add("dve", lambda h: h.tensor_tensor(out=cbm[gb][:], in0=sg_[:, 384:512], in1=mJL[:], op=ALU.mult),
                          r=[sgr], w=["cbm%d" % gb])
                    S.add("dve", lambda h: h.tensor_tensor(
                        out=mt[gb][:], in0=sg_[:, 0:384].rearrange("p (a b) -> p a b", b=128),
                        in1=cbm[gb][:, None, :].to_broadcast([128, 3, 128]), op=ALU.mult),
                        r=[sgr, "cbm%d" % gb], w=["mt%d" % gb])

                def stage_q(g):
                    gb = g % 2
                    py = psY[:, gb * 512:(gb + 1) * 512]
                    pyr = "psY%d" % gb
                    for r_ in range(3):
                        hh = 3 * g + r_
                        S.add("pe", lambda h: h.matmul(
                            py[:, r_ * 64:(r_ + 1) * 64], lhsT=mt[gb][:, r_, :], rhs=xc[:, hh * 64:(hh + 1) * 64],
                            start=True, stop=False), r=["mt%d" % gb, "xc"], w=[pyr])
                        S.add("pe", lambda h: h.matmul(
                            py[:, r_ * 64:(r_ + 1) * 64], lhsT=ddiag[:, hh, :], rhs=xtm[:, hh * 64:(hh + 1) * 64],
                            start=False, stop=True), r=["xtm"], w=[pyr])
                    S.add("pe", lambda h: h.matmul(py[:, 192:384], lhsT=xact[:, 10 + g, cs], rhs=Sbf[:, g, :],
                                                   start=True, stop=True), r=["xact", "Sbf%d" % g], w=[pyr])
                    S.add("pe", lambda h: h.matmul(psS[:, 192:384], lhsT=btm[:, g * 128:(g + 1) * 128],
                                                   rhs=xdte[:, g * 192:(g + 1) * 192], start=True, stop=True),
                          r=["btm", "xdte"], w=["psS"])
                    S.add("dve", lambda h: h.tensor_tensor(
                        out=ytmp[gb][:].rearrange("p (a b) -> p a b", b=64), in0=py[:, 192:384].rearrange("p (a b) -> p a b", b=64),
                        in1=e24[:, 3 * g:3 * g + 3].unsqueeze(2).to_broadcast([128, 3, 64]), op=ALU.mult),
                        r=[pyr, "e24"], w=["ytmp%d" % gb])
                    S.add("dve", lambda h: h.tensor_tensor(out=ych[:, g * 192:(g + 1) * 192], in0=py[:, 0:192],
                                                           in1=ytmp[gb][:], op=ALU.add), r=[pyr, "ytmp%d" % gb], w=["ych%d" % g])
                    S.add("pool", lambda h: h.tensor_tensor(
                        out=Sst[:, g, :].rearrange("p (a b) -> p a b", b=64), in0=Sst[:, g, :].rearrange("p (a b) -> p a b", b=64),
                        in1=e24[:, 12 + 3 * g:15 + 3 * g].unsqueeze(2).to_broadcast([128, 3, 64]), op=ALU.mult),
                        r=["Sst%d" % g, "e24"], w=["Sst%d" % g])
                    S.add("dve", lambda h: h.tensor_tensor(out=Sst[:, g, :], in0=psS[:, 192:384], in1=Sst[:, g, :], op=ALU.add),
                          r=["psS", "Sst%d" % g], w=["Sst%d" % g])
                    S.add("pool", lambda h: h.tensor_copy(out=Sbf[:, g, :], in_=Sst[:, g, :]), r=["Sst%d" % g], w=["Sbf%d" % g])

                stage_p(0)
                stage_p(1)
                stage_q(0)
                stage_p(2)
                stage_q(1)
                stage_p(3)
                stage_q(2)
                stage_q(3)
                if c == 0 and "C3" in debug:
                    S.barrier()
                    dump('ych', ych[:], [128, 768], [])
                    return finish()
                S.add("pool", lambda h: h.tensor_tensor(out=ych[:], in0=ych[:], in1=sz[:], op=ALU.mult), r=["ych0", "ych1", "ych2", "ych3"] + ["sz"], w=["ych"])
                for g in range(4):
                    S.add("act", lambda h, g=g: h.activation(out=junk[:, 0:192], in_=ych[:, g * 192:(g + 1) * 192], func=SQ,
                                                             accum_out=stat[:, 4 + g:5 + g]), r=["ych", "ych0", "ych1", "ych2", "ych3", "junk"], w=["junk", "stat4"])
                rstd_ops(stat[:, 4:8], stat[:, 8:12], 192, "stat4", "stat8")
                S.add("dve", lambda h: h.tensor_tensor(out=ygb[:].rearrange("p (a b) -> p a b", b=192),
                                                       in0=ych[:].rearrange("p (a b) -> p a b", b=192),
                                                       in1=stat[:, 8:12].unsqueeze(2).to_broadcast([128, 4, 192]), op=ALU.mult),
                      r=["ych", "ych0", "ych1", "ych2", "ych3", "stat8"], w=["ygb"])
                for b6 in range(6):
                    S.add("pe", lambda h, b6=b6: h.transpose(psT_bf[:, b6 * 128:(b6 + 1) * 128], ygb[:, b6 * 128:(b6 + 1) * 128], ident_b[:]),
                          r=["ygb"], w=["psT"])
                S.add("dve", lambda h: h.tensor_tensor(out=mixT[:, 2:8, cs], in0=psT_bf[:, 0:768].rearrange("p (a b) -> p a b", b=128),
                                                       in1=normw[:, :].unsqueeze(2).to_broadcast([128, 6, 128]), op=ALU.mult),
                      r=["psT"], w=["mixT_s"])
                if c == 0 and "C4" in debug:
                    S.barrier()
                    dump('mixTs', mixT[:, 2:8, 0:128], [128, 6, 128], [], BF16)
                    return finish()
                if c == 0:
                    pool_mm()
                for half in range(2):
                    for kc in range(KC):
                        S.add("pe", lambda h, half=half, kc=kc: h.matmul(
                            psW[:, half * 512:(half + 1) * 512], lhsT=mixT[:, kc, cs], rhs=wout_b[:, kc, half * 512:(half + 1) * 512],
                            start=(kc == 0), stop=(kc == KC - 1)), r=["mixT_s", "mixT_p"], w=["psW%d" % half])
                S.add("dve", lambda h: h.tensor_tensor(out=x1[:], in0=psW[:, :], in1=repA[:, 0, :], op=ALU.mult),
                      r=["psW0", "psW1", "repA"], w=["x1"])
                S.add("pool", lambda h, xs_=xs_: h.tensor_tensor(out=x1[:], in0=x1[:], in1=xs_[:], op=ALU.add),
                      r=["x1", xr], w=["x1"])
                row0 = tok0 + c * 128
                dma("sp", x1s_d[row0:row0 + 128, :], x1[:], ["x1"], [], "x1st")
                if c == 0 and "C5" in debug:
                    S.barrier()
                    dump('x1', x1[:], [128, D], [])
                    return finish()
                S.add("act", lambda h: h.activation(out=junk[:], in_=x1[:], func=SQ, accum_out=stat[:, 2:3]),
                      r=["x1", "junk"], w=["junk", "stat2"])
                rstd_ops(stat[:, 2:3], stat[:, 3:4], D, "stat2", "stat3")
                S.add("act", lambda h: h.activation(out=xn2[:], in_=x1[:], func=COPY, scale=stat[:, 3:4]),
                      r=["x1", "stat3"], w=["xn2"])
                S.add("pool", lambda h: h.tensor_tensor(out=h2t[:], in0=xn2[:], in1=repA[:, 2, :], op=ALU.mult), r=["xn2", "repA"], w=["h2t"])
                S.add("pool", lambda h: h.tensor_tensor(out=h2b[:], in0=h2t[:], in1=repA[:, 1, :], op=ALU.add),
                      r=["h2t", "repA"], w=["h2b"])
                dma("sp", h2s_d[row0:row0 + 128, :], h2b[:], ["h2b"], [], "h2st")
                for kc in range(KC):
                    S.add("pe", lambda h, kc=kc: h.transpose(psSG[:, kc * 128:(kc + 1) * 128], xn2[:, kc * 128:(kc + 1) * 128], ident_f[:]),
                          r=["xn2"], w=["psSG%d" % (kc // 4)])
                for kc in range(KC):
                    S.add("dve", lambda h, kc=kc: h.tensor_scalar(
                        out=h2T[:, kc, :], in0=psSG[:, kc * 128:(kc + 1) * 128], scalar1=modp[:, 32 + kc, s:s + 1],
                        scalar2=modp[:, 24 + kc, s:s + 1], op0=ALU.mult, op1=ALU.add),
                        r=["psSG%d" % (kc // 4)], w=["h2T"])
                for kc in range(KC):
                    S.add("pe", lambda h, kc=kc: h.matmul(psS[:, 40:76], lhsT=h2T[:, kc, :], rhs=wgr[:, kc, :],
                                                           start=(kc == 0), stop=(kc == KC - 1)), r=["h2T"], w=["psS"])
                if c == 0 and "C6" in debug:
                    S.barrier()
                    dump('h2b', h2b[:], [128, D], [], BF16)
                    dump('h2T', h2T[:], [128, KC, 128], [])
                    return finish()
                lg = rt[:, 0, :]
                me = rt[:, 1, 0:32]
                me2 = rt[:, 2, 0:32]
                sc_ = rt[:, 3, :]
                goh = rt[:, 4, 0:4]
                pen = rt[:, 4, 4:8]
                gs = rt[:, 4, 8:12]
                oh1 = rt[:, 5, 0:32]
                oh2 = rt[:, 6, 0:32]
                tm_ = rt[:, 7, 0:32]
                V = "dve"
                RT = ["rt"]
                S.add(V, lambda h: h.tensor_tensor(out=lg, in0=psS[:, 40:76], in1=bgr[:], op=ALU.add), r=["psS"], w=RT)
                S.add(V, lambda h: h.tensor_reduce(out=sc_[:, 0:1], in_=lg[:, 0:4], axis=AX.X, op=ALU.max), r=RT, w=RT)
                S.add(V, lambda h: h.tensor_scalar(out=gs, in0=lg[:, 0:4], scalar1=sc_[:, 0:1], scalar2=None, op0=ALU.subtract), r=RT, w=RT)
                S.add("act", lambda h: h.activation(out=gs, in_=gs, func=EXP, accum_out=sc_[:, 1:2]), r=RT, w=RT)
                S.add(V, lambda h: h.reciprocal(out=sc_[:, 2:3], in_=sc_[:, 1:2]), r=RT, w=RT)
                S.add(V, lambda h: h.tensor_scalar(out=goh, in0=lg[:, 0:4], scalar1=sc_[:, 0:1], scalar2=None, op0=ALU.is_equal), r=RT, w=RT)
                S.add(V, lambda h: h.tensor_scalar(out=pen, in0=goh, scalar1=BIG, scalar2=-BIG, op0=ALU.mult, op1=ALU.add), r=RT, w=RT)
                S.add(V, lambda h: h.tensor_tensor(out=me.rearrange("p (a b) -> p a b", b=8), in0=lg[:, 4:36].rearrange("p (a b) -> p a b", b=8),
                                                   in1=pen.unsqueeze(2).to_broadcast([128, 4, 8]), op=ALU.add), r=RT, w=RT)
                S.add(V, lambda h: h.tensor_reduce(out=sc_[:, 3:4], in_=me, axis=AX.X, op=ALU.max), r=RT, w=RT)
                S.add(V, lambda h: h.tensor_scalar(out=oh1, in0=me, scalar1=sc_[:, 3:4], scalar2=None, op0=ALU.is_equal), r=RT, w=RT)
                S.add(V, lambda h: h.scalar_tensor_tensor(out=me2, in0=oh1, scalar=-BIG, in1=me, op0=ALU.mult, op1=ALU.add), r=RT, w=RT)
                S.add(V, lambda h: h.tensor_reduce(out=sc_[:, 4:5], in_=me2, axis=AX.X, op=ALU.max), r=RT, w=RT)
                S.add(V, lambda h: h.tensor_scalar(out=oh2, in0=me2, scalar1=sc_[:, 4:5], scalar2=None, op0=ALU.is_equal), r=RT, w=RT)
                S.add(V, lambda h: h.tensor_tensor(out=sc_[:, 5:6], in0=sc_[:, 4:5], in1=sc_[:, 3:4], op=ALU.subtract), r=RT, w=RT)
                S.add("act", lambda h: h.activation(out=sc_[:, 6:7], in_=sc_[:, 5:6], func=EXP), r=RT, w=RT)
                S.add(V, lambda h: h.tensor_scalar(out=sc_[:, 7:8], in0=sc_[:, 6:7], scalar1=1.0, scalar2=None, op0=ALU.add), r=RT, w=RT)
                S.add(V, lambda h: h.reciprocal(out=sc_[:, 8:9], in_=sc_[:, 7:8]), r=RT, w=RT)
                S.add(V, lambda h, ti=ti: h.tensor_tensor(out=gates[:, ti, 0:1], in0=sc_[:, 8:9], in1=sc_[:, 2:3], op=ALU.mult), r=RT, w=["gates"])
                S.add(V, lambda h, ti=ti: h.tensor_tensor(out=gates[:, ti, 1:2], in0=gates[:, ti, 0:1], in1=sc_[:, 6:7], op=ALU.mult), r=RT + ["gates"], w=["gates"])
                S.add(V, lambda h: h.tensor_tensor(out=abf[:], in0=oh1, in1=oh2, op=ALU.add), r=RT, w=["abf"])
                S.add("pe", lambda h: h.matmul(psS[:, 76:108], lhsT=mLT_b[:], rhs=abf[:], start=True, stop=True), r=["abf"], w=["psS"])
                S.add("pe", lambda h: h.matmul(psS[:, 108:140], lhsT=ones_b[:], rhs=abf[:], start=True, stop=True), r=["abf"], w=["psS"])
                for k, oh in ((0, oh1), (1, oh2)):
                    S.add(V, lambda h, oh=oh: h.tensor_tensor(out=tm_, in0=oh, in1=iota_e[:], op=ALU.mult), r=RT, w=RT)
                    S.add(V, lambda h, k=k, ti=ti: h.tensor_reduce(out=eidx[:, ti, k:k + 1], in_=tm_, axis=AX.X, op=ALU.add), r=RT, w=["eidx"])
                S.add(V, lambda h: h.tensor_tensor(out=me, in0=psS[:, 76:108], in1=totrun[:], op=ALU.add), r=["psS", "totrun"] + RT, w=RT)
                for k, oh in ((0, oh1), (1, oh2)):
                    S.add(V, lambda h, oh=oh: h.tensor_tensor(out=tm_, in0=oh, in1=me, op=ALU.mult), r=RT, w=RT)
                    S.add(V, lambda h, k=k, ti=ti: h.tensor_reduce(out=posk[:, ti, k:k + 1], in_=tm_, axis=AX.X, op=ALU.add), r=RT, w=["posk"])
                S.add(V, lambda h: h.tensor_tensor(out=totrun[:], in0=psS[:, 108:140], in1=totrun[:], op=ALU.add), r=["psS", "totrun"], w=["totrun"])
            tile_i += CPM

    if "A" in debug:
        S.barrier()
        dump("modp", modp[:], [128, 48, nseq], [])
        dump("gates", gates[:], [128, NT, 2], [])
        dump("eidx", eidx[:], [128, NT, 2], [])
        dump("posk", posk[:], [128, NT, 2], [])
        dump("hT", hT[:], [128, KC, TM], [], BF16)
        dump("xact", xact[:], [128, 14, TM], [], BF16)
        dump("mixTs", mixT[:, 2:8, :], [128, 6, TM], [], BF16)
        dump("mixTp", mixT[:, 0:2, :], [128, 2, TM], [], BF16)
        dump("ych", ych[:], [128, 768], [])
        dump("x1", x1[:], [128, D], [])
        dump("e24", e24[:], [128, 24], [])
        dump("sz", sz[:], [128, 768], [])
        dump("h2b", h2b[:], [128, D], [], BF16)
        return finish()

    S.barrier()
    stA.close()
    stB = ExitStack()

    g2rep = sb("g2rep", [128, nseq, D], stack=stB)
    stB1 = ExitStack()
    curB = [stB1]

    def sbb(name, shape, dt=F32):
        return sb(name, shape, dt, stack=curB[0])

    thr = sbb("thr", [128, 128])
    bst = sbb("bst", [128, NB])
    cmpb = sbb("cmpb", [128, 32 * 128])
    cmp2 = sbb("cmp2", [128, NB * 32])
    ohb = sbb("ohb", [128, NT * 32])
    nbk = sbb("nbk", [128, 32])
    ca = sbb("ca", [128, 32])
    cb_ = sbb("cb_", [128, 32])
    pstart = sbb("pstart", [128, 32])
    blke = sbb("blke", [128, NB])
    widx_f = sbb("widx_f", [128, NB])
    eqf = sbb("eqf", [128, NB])
    dst_f = sbb("dst_f", [128, NT, 2])
    pst = sbb("pst", [128, NT])
    dg = sbb("dg2", [128, 4, 128])

    S.add("pool", lambda h: h.iota(thr[:], pattern=[[128, 128]], base=0, channel_multiplier=0,
                                   allow_small_or_imprecise_dtypes=True), w=["thr"])
    S.add("pool", lambda h: h.iota(bst[:], pattern=[[128, NB]], base=0, channel_multiplier=0,
                                   allow_small_or_imprecise_dtypes=True), w=["bst"])
    V = "dve"
    S.add(V, lambda h: h.tensor_tensor(out=cmpb[:].rearrange("p (a b) -> p a b", b=128),
                                       in0=totrun[:, :].unsqueeze(2).to_broadcast([128, 32, 128]),
                                       in1=thr[:, None, :].to_broadcast([128, 32, 128]), op=ALU.is_gt),
          r=["thr", "totrun"], w=["cmpb"])
    S.add(V, lambda h: h.tensor_reduce(out=nbk[:], in_=cmpb[:].rearrange("p (a b) -> p a b", b=128), axis=AX.X, op=ALU.add),
          r=["cmpb"], w=["nbk"])
    S.add(V, lambda h: h.tensor_scalar(out=nbk[:], in0=nbk[:], scalar1=128.0, scalar2=None, op0=ALU.mult), r=["nbk"], w=["nbk"])
    S.add(V, lambda h: h.tensor_copy(out=ca[:], in_=nbk[:]), r=["nbk"], w=["ca"])
    a_, b_ = ca, cb_
    an, bn = "ca", "cb_"
    for sh in (1, 2, 4, 8, 16):
        S.add(V, lambda h, a_=a_, b_=b_, sh=sh: h.tensor_tensor(out=b_[:, sh:32], in0=a_[:, sh:32], in1=a_[:, 0:32 - sh], op=ALU.add),
              r=[an], w=[bn])
        S.add(V, lambda h, a_=a_, b_=b_, sh=sh: h.tensor_copy(out=b_[:, 0:sh], in_=a_[:, 0:sh]), r=[an, bn], w=[bn])
        a_, b_, an, bn = b_, a_, bn, an
    pends, pn = a_, an
    S.add(V, lambda h: h.tensor_tensor(out=pstart[:], in0=pends[:], in1=nbk[:], op=ALU.subtract), r=[pn, "nbk"], w=["pstart"])
    S.add(V, lambda h: h.tensor_tensor(out=cmp2[:].rearrange("p (a b) -> p a b", b=32),
                                       in0=pends[:, None, :].to_broadcast([128, NB, 32]),
                                       in1=bst[:, :].unsqueeze(2).to_broadcast([128, NB, 32]), op=ALU.is_le),
          r=[pn, "bst"], w=["cmp2"])
    S.add(V, lambda h: h.tensor_reduce(out=blke[:], in_=cmp2[:].rearrange("p (a b) -> p a b", b=32), axis=AX.X, op=ALU.add),
          r=["cmp2"], w=["blke"])
    S.add(V, lambda h: h.tensor_scalar(out=blke[:], in0=blke[:], scalar1=31.0, scalar2=None, op0=ALU.min), r=["blke"], w=["blke"])
    S.add(V, lambda h: h.tensor_scalar(out=widx_f[:], in0=blke[:], scalar1=128.0, scalar2=iota_p[:, 0:1], op0=ALU.mult, op1=ALU.add),
          r=["blke"], w=["widx_f"])
    S.add(V, lambda h: h.tensor_tensor(out=eqf[:, 2:NB], in0=blke[:, 2:NB], in1=blke[:, 0:NB - 2], op=ALU.is_equal), r=["blke"], w=["eqf"])
    S.add(V, lambda h: h.memset(eqf[:, 0:2], 0.0), r=["eqf"], w=["eqf"])
    for nm, kk, dst in (("w13f", 4, widx13_i), ("w2f", 2, widx2_i)):
        wf = sbb(nm, [128, NB, kk])
        bk = sbb(nm + "b", [128, NB])
        S.add(V, lambda h, bk=bk, kk=kk: h.tensor_scalar(out=bk[:], in0=widx_f[:], scalar1=float(kk), scalar2=None, op0=ALU.mult),
              r=["widx_f"], w=[nm + "b"])
        S.add(V, lambda h, bk=bk: h.scalar_tensor_tensor(out=bk[:], in0=eqf[:], scalar=40000.0, in1=bk[:], op0=ALU.mult, op1=ALU.add),
              r=["eqf", nm + "b"], w=[nm + "b"])
        S.add(V, lambda h, bk=bk, wf=wf, kk=kk: h.tensor_tensor(out=wf[:], in0=bk[:, :].unsqueeze(2).to_broadcast([128, NB, kk]),
                                                                in1=iota_e[:, None, 0:kk].to_broadcast([128, NB, kk]), op=ALU.add),
              r=[nm + "b"], w=[nm])
        S.add(V, lambda h, wf=wf, dst=dst: h.tensor_copy(out=dst[:], in_=wf[:]), r=[nm], w=["widx_i"])
    for k in range(2):
        S.add(V, lambda h, k=k: h.tensor_tensor(out=ohb[:].rearrange("p (a b) -> p a b", b=32),
                                               in0=iota_e[:, None, :].to_broadcast([128, NT, 32]),
                                               in1=eidx[:, :, k:k + 1].to_broadcast([128, NT, 32]), op=ALU.is_equal),
              r=["eidx", "pst"], w=["ohb"])
        S.add(V, lambda h: h.tensor_tensor(out=ohb[:].rearrange("p (a b) -> p a b", b=32),
                                           in0=ohb[:].rearrange("p (a b) -> p a b", b=32),
                                           in1=pstart[:, None, :].to_broadcast([128, NT, 32]), op=ALU.mult),
              r=["ohb", "pstart"], w=["ohb"])
        S.add(V, lambda h: h.tensor_reduce(out=pst[:], in_=ohb[:].rearrange("p (a b) -> p a b", b=32), axis=AX.X, op=ALU.add),
              r=["ohb"], w=["pst"])
        S.add(V, lambda h, k=k: h.tensor_tensor(out=dst_f[:, :, k:k + 1], in0=posk[:, :, k:k + 1], in1=pst[:, :].unsqueeze(2), op=ALU.add),
              r=["pst", "posk"], w=["dst_f"])
    S.add(V, lambda h: h.tensor_copy(out=dest_i[:], in_=dst_f[:]), r=["dst_f"], w=["dest_i"])
    for s in range(nseq):
        for q in range(2):
            S.add("dve", lambda h, q=q, s=s: h.tensor_tensor(
                out=dg[:], in0=ident_f[:, None, :].to_broadcast([128, 4, 128]),
                in1=modp[:, 40 + 4 * q:44 + 4 * q, s:s + 1].to_broadcast([128, 4, 128]), op=ALU.mult),
                r=[], w=["dg2"])
            S.add("pe", lambda h: h.matmul(psW[:, 0:512], lhsT=ones_f[:], rhs=dg[:].rearrange("p a b -> p (a b)"),
                                           start=True, stop=True), r=["dg2"], w=["psW0"])
            S.add("act", lambda h, q=q, s=s: h.activation(out=g2rep[:, s, q * 512:(q + 1) * 512], in_=psW[:, 0:512], func=COPY),
                  r=["psW0"], w=["g2rep"])

    if "B" in debug:
        S.barrier()
        dump("widx", widx13_i[:], [128, NB, 4], [], I32)
        dump("dest", dest_i[:], [128, NT, 2], [], I32)
        dump("blke", blke[:], [128, NB], [])
        dump("pstart", pstart[:], [128, 32], [])
        dump("totrun", totrun[:], [128, 32], [])
        return finish()

    hb = [sbb("hb%d" % i, [128, D], BF16) for i in range(2)]
    zt = sbb("zt", [128, 8, D], BF16)
    S.add("pool", lambda h: h.memset(zt[:], 0.0), r=[], w=["zt"])
    b0 = 0
    while b0 < NB:
        nb_ = min(8, NB - b0)
        dma("sp", xs_d[b0 * 128:(b0 + nb_) * 128, :].rearrange("(n p) d -> p n d", p=128), zt[:, 0:nb_, :], ["zt"], [], "zfill")
        b0 += nb_
    S.barrier()
    for t in range(NT):
        sl = t % 2
        dma("sp", hb[sl][:], h2s_d[t * 128:(t + 1) * 128, :], [], ["hb%d" % sl], "hbl%d" % sl)
        for k in range(2):
            S.add("pool", lambda h, t=t, k=k, sl=sl: h.indirect_dma_start(
                out=xs_d[:, :], out_offset=bass.IndirectOffsetOnAxis(ap=dest_i[:, t, k:k + 1], axis=0),
                in_=hb[sl][:], in_offset=None), r=["hb%d" % sl, "dest_i"], w=[], dma="sc%d" % sl)
    S.barrier()

    stB1.close()
    curB[0] = stB
    W13 = [sbb("W13_%d" % i, [128, 8, 1024], BF16) for i in range(2)]
    W2 = [sbb("W2_%d" % i, [128, 4, 1024], BF16) for i in range(2)]
    xb = [sbb("xb%d" % i, [128, D], BF16) for i in range(2)]
    xT = [sbb("xT%d" % i, [128, 8, 128], BF16) for i in range(2)]
    sa = [sbb("sa%d" % i, [128, 512]) for i in range(2)]
    actb = [sbb("actb%d" % i, [128, 512], BF16) for i in range(2)]
    actT = [sbb("actT%d" % i, [128, 4, 128], BF16) for i in range(2)]
    yb = [sbb("yb%d" % i, [128, D]) for i in range(2)]
    reg13 = nc.gpsimd.alloc_register("bc13")
    reg2 = nc.gpsimd.alloc_register("bc2")
    S.add("pool", lambda h: h.reg_mov(reg13, NE * 512 - 1), r=[], w=[])
    S.add("pool", lambda h: h.reg_mov(reg2, NE * 256 - 1), r=[], w=[])
    w13v = w13_d.rearrange("e (r two) n -> (e r) (two n)", two=2)
    w2v = w2_d.rearrange("e (r two) n -> (e r) (two n)", two=2)
    psH = [psW, psSG]
    psHn = [("psW0", "psW1"), ("psSG0", "psSG1")]

    def front(b):
        sl = b % 2
        for j in range(4):
            S.add("pool", lambda h: h.indirect_dma_start(
                out=W13[sl][:, 2 * j:2 * j + 2, :].rearrange("p a n -> p (a n)"), out_offset=None, in_=w13v,
                in_offset=bass.IndirectOffsetOnAxis(ap=widx13_i[:, b, j:j + 1], axis=0),
                bounds_check=reg13, oob_is_err=False), r=["widx_i"], w=["W13_%d_%d" % (sl, j)], dma="w13_%d" % sl)
        for j in range(2):
            S.add("pool", lambda h: h.indirect_dma_start(
                out=W2[sl][:, 2 * j:2 * j + 2, :].rearrange("p a n -> p (a n)"), out_offset=None, in_=w2v,
                in_offset=bass.IndirectOffsetOnAxis(ap=widx2_i[:, b, j:j + 1], axis=0),
                bounds_check=reg2, oob_is_err=False), r=["widx_i"], w=["W2_%d_%d" % (sl, j)], dma="w2_%d" % sl)
        dma("sp", xb[sl][:], xs_d[b * 128:(b + 1) * 128, :], [], ["xb%d" % sl], "xbl%d" % sl)
        for kc in range(8):
            S.add("pe", lambda h: h.transpose(psT_bf[:, kc * 128:(kc + 1) * 128], xb[sl][:, kc::8], ident_b[:]),
                  r=["xb%d" % sl], w=["psT"])
        S.add("dve", lambda h: h.tensor_scalar(out=xT[sl][:].rearrange("p a b -> p (a b)"), in0=psT_bf[:, :], scalar1=1.0,
                                               scalar2=None, op0=ALU.mult), r=["psT"], w=["xT%d" % sl])
        ph = psH[sl]
        for half in range(2):
            for kc in range(8):
                S.add("pe", lambda h: h.matmul(
                    ph[:, half * 512:(half + 1) * 512], lhsT=xT[sl][:, kc, :], rhs=W13[sl][:, kc, half * 512:(half + 1) * 512],
                    start=(kc == 0), stop=(kc == 7)), r=["xT%d" % sl] + ["W13_%d_%d" % (sl, k_) for k_ in range(4)],
                    w=[psHn[sl][half]])
        S.add("act", lambda h: h.activation(out=sa[sl][:], in_=ph[:, 0:512], func=SILU), r=[psHn[sl][0]], w=["sa%d" % sl])
        S.add("dve", lambda h: h.tensor_tensor(out=actb[sl][:], in0=ph[:, 512:1024], in1=sa[sl][:], op=ALU.mult),
              r=[psHn[sl][1], "sa%d" % sl], w=["actb%d" % sl])

    def back(b):
        sl = b % 2
        for fc in range(4):
            S.add("pe", lambda h: h.transpose(psT_bf[:, fc * 128:(fc + 1) * 128], actb[sl][:, fc::4], ident_b[:]),
                  r=["actb%d" % sl], w=["psT"])
        S.add("dve", lambda h: h.tensor_scalar(out=actT[sl][:].rearrange("p a b -> p (a b)"), in0=psT_bf[:, 0:512], scalar1=1.0,
                                               scalar2=None, op0=ALU.mult), r=["psT"], w=["actT%d" % sl])
        for half in range(2):
            for fc in range(4):
                S.add("pe", lambda h: h.matmul(
                    psY[:, half * 512:(half + 1) * 512], lhsT=actT[sl][:, fc, :], rhs=W2[sl][:, fc, half * 512:(half + 1) * 512],
                    start=(fc == 0), stop=(fc == 3)), r=["actT%d" % sl] + ["W2_%d_%d" % (sl, k_) for k_ in range(2)],
                    w=["psY%d" % half])
        S.add("act", lambda h: h.activation(out=yb[sl][:, 0:512], in_=psY[:, 0:512], func=COPY), r=["psY0"], w=["yb%da" % sl])
        S.add("act", lambda h: h.activation(out=yb[sl][:, 512:1024], in_=psY[:, 512:1024], func=COPY), r=["psY1"], w=["yb%db" % sl])
        dma("sp", ys_d[b * 128:(b + 1) * 128, :], yb[sl][:], ["yb%da" % sl, "yb%db" % sl], [], "yst%d" % sl)

    front(0)
    for b in range(NB):
        if b + 1 < NB:
            front(b + 1)
        back(b)
    S.barrier()

    y1 = [sbb("y1_%d" % i, [128, D]) for i in range(2)]
    y2 = [sbb("y2_%d" % i, [128, D]) for i in range(2)]
    x1l = [sbb("x1l%d" % i, [128, D]) for i in range(2)]
    mm = sbb("mm", [128, D])
    oo = [sbb("oo%d" % i, [128, D]) for i in range(2)]
    junk2 = sbb("junk2", [128, D], BF16)
    st2 = sbb("st2", [128, 4])
    for t in range(NT):
        sl = t % 2
        sq = (t * 128) // seqlen
        for k, yy in ((0, y1), (1, y2)):
            S.add("pool", lambda h, t=t, k=k, yy=yy, sl=sl: h.indirect_dma_start(
                out=yy[sl][:], out_offset=None, in_=ys_d[:, :],
                in_offset=bass.IndirectOffsetOnAxis(ap=dest_i[:, t, k:k + 1], axis=0)),
                r=["dest_i"], w=["y%d_%d" % (k, sl)], dma="yg%d_%d" % (k, sl))
        dma("sp", x1l[sl][:], x1s_d[t * 128:(t + 1) * 128, :], [], ["x1l%d" % sl], "x1ld%d" % sl)
        S.add("dve", lambda h, t=t, sl=sl: h.tensor_scalar(out=mm[:], in0=y1[sl][:], scalar1=gates[:, t, 0:1], scalar2=None, op0=ALU.mult),
              r=["y0_%d" % sl], w=["mm"])
        S.add("dve", lambda h, t=t, sl=sl: h.scalar_tensor_tensor(out=mm[:], in0=y2[sl][:], scalar=gates[:, t, 1:2], in1=mm[:],
                                                                  op0=ALU.mult, op1=ALU.add), r=["y1_%d" % sl, "mm"], w=["mm"])
        S.add("pool", lambda h, sq=sq: h.tensor_tensor(out=mm[:], in0=mm[:], in1=g2rep[:, sq, :], op=ALU.mult), r=["mm", "g2rep"], w=["mm"])
        S.add("pool", lambda h, sl=sl: h.tensor_tensor(out=mm[:], in0=mm[:], in1=x1l[sl][:], op=ALU.add), r=["mm", "x1l%d" % sl], w=["mm"])
        S.add("act", lambda h: h.activation(out=junk2[:], in_=mm[:], func=SQ, accum_out=st2[:, 0:1]), r=["mm", "junk2"], w=["junk2", "st2a"])
        rstd_ops(st2[:, 0:1], st2[:, 1:2], D, "st2a", "st2b")
        S.add("dve", lambda h, sl=sl: h.scalar_tensor_tensor(out=oo[sl][:], in0=mm[:], scalar=st2[:, 1:2], in1=fwrep[:],
                                                             op0=ALU.mult, op1=ALU.mult), r=["mm", "st2b"], w=["oo%d" % sl])
        dma("sp", out_d[t * 128:(t + 1) * 128, :], oo[sl][:], ["oo%d" % sl], [], "ost%d" % sl)
    return finish()


_CACHE = {}


def _prep_common(inp):
    f = np.float32
    w_in = np.asarray(inp["w_in"], f)[0]
    perm = np.concatenate([np.arange(0, 256), np.arange(1024, 2816), np.arange(256, 1024), np.arange(2816, 2828)])
    cw = np.asarray(inp["conv_w"], f)[0]
    grp = np.array([[2 * b + (1 if p >= 64 else 0) for b in range(2)] for p in range(128)])
    wins = np.array([2, 4, 8, 16])[grp]
    pcoef = np.zeros((128, 2, 4), f)
    for wi, w in enumerate((2, 4, 8, 16)):
        pcoef[:, :, wi] = np.where(wins == w, 1.0 / w, 0.0)
    pos = np.arange(1, 17, dtype=f)[None, None, :]
    pratio = (wins[:, :, None] / np.minimum(pos, wins[:, :, None])).astype(f)
    rep = lambda v: np.ascontiguousarray(np.broadcast_to(np.asarray(v, f).reshape(1, -1), (128, np.asarray(v).size)))
    return {
        "w_ada": np.ascontiguousarray(np.asarray(inp["w_ada"], f)[0]),
        "b_adaT": np.ascontiguousarray(np.asarray(inp["b_ada"], f)[0].reshape(48, 128).T),
        "w_in": np.ascontiguousarray(w_in[:, perm]),
        "w_pool": np.ascontiguousarray(np.asarray(inp["w_pool"], f)[0]),
        "pscale": np.ascontiguousarray(np.asarray(inp["pool_scale"], f)[0].reshape(2, 128).T),
        "convw": np.ascontiguousarray(cw.T.reshape(14, 128, 4).transpose(1, 0, 2).reshape(128, 56)),
        "convb": np.ascontiguousarray(np.asarray(inp["conv_b"], f)[0].reshape(14, 128).T),
        "dtb": rep(inp["dt_bias"]), "alog": rep(inp["a_log"]), "dskip": rep(inp["d_skip"]),
        "normw": np.ascontiguousarray(np.asarray(inp["ssd_norm_w"], f)[0].reshape(6, 128).T),
        "w_out": np.ascontiguousarray(np.asarray(inp["w_out"], f)[0]),
        "wgr": np.ascontiguousarray(np.concatenate([np.asarray(inp["w_group"], f)[0], np.asarray(inp["w_router"], f)[0]], axis=1)
                                    .reshape(8, 128, 36).transpose(1, 0, 2)),
        "bgr": rep(np.concatenate([np.asarray(inp["b_group"], f)[0], np.asarray(inp["b_router"], f)[0]])),
        "w13": np.ascontiguousarray(np.asarray(inp["w13"], f)[0]),
        "w2": np.ascontiguousarray(np.asarray(inp["w2"], f)[0]),
        "fw": rep(inp["final_norm_w"]),
        "pcoef": pcoef, "pratio": pratio,
    }


def _core_map(common, x, c, nseq, seqlen, i):
    f = np.float32
    xc_ = np.ascontiguousarray(np.asarray(x[i * nseq:(i + 1) * nseq], f).reshape(nseq * seqlen, D))
    cc = np.asarray(c[i * nseq:(i + 1) * nseq], f)
    cT = np.ascontiguousarray(cc.T.reshape(8, 128, nseq).transpose(1, 0, 2))
    m = dict(common)
    m["x"] = xc_
    m["cT"] = cT
    return m


def kernel(**inputs):
    x = np.asarray(inputs["x"])
    c = np.asarray(inputs["c"])
    B, SEQ, _ = x.shape
    nseq = B // NCORES
    key = (nseq, SEQ)
    if key not in _CACHE:
        _CACHE[key] = build(nseq, SEQ)[0]
    nc = _CACHE[key]
    common = _prep_common(inputs)
    in_maps = [_core_map(common, x, c, nseq, SEQ, i) for i in range(NCORES)]
    res = run_bass_kernel_spmd(nc, in_maps, core_ids=list(range(NCORES)))
    outs = [np.asarray(r["out"]).reshape(nseq, SEQ, D) for r in res.results]
    return np.concatenate(outs, axis=0).astype(np.float32)
```
